# Optimizing a Trainium2 kernel written in Bass

```python
import math
import jax, jax.numpy as jnp
from jax import lax
import numpy as np

D_MODEL = 1024
BATCH = 4
SEQ = 8192
DEPTH = 2

EPS = 1e-6
CHUNK = 64
Q_BLOCK = 128
CONV_K = 4
GLA_HEADS = 4
GLA_DK = 64
GLA_DV = 128
GLA_GATE_RANK = 16
GLA_GATE_NORM = 16.0
SSD_HEADS = 16
SSD_HEADDIM = 64
SSD_GROUPS = 2
SSD_HPG = SSD_HEADS // SSD_GROUPS
SSD_STATE = 128
SSD_INNER = SSD_HEADS * SSD_HEADDIM
SSD_CONV_DIM = SSD_INNER + 2 * SSD_GROUPS * SSD_STATE
LRU_WIDTH = D_MODEL
LRU_BLOCKS = 16
LRU_BLOCK_W = LRU_WIDTH // LRU_BLOCKS
LRU_C = 8.0
MLA_HEADS = 8
MLA_NOPE = 128
MLA_ROPE = 64
MLA_QK_DIM = MLA_NOPE + MLA_ROPE
MLA_V = 128
MLA_Q_LORA = 384
MLA_KV_LORA = 256
ROPE_THETA = 10000.0
N_EXPERTS = 16
N_EXPERT_GROUPS = 4
EXPERTS_PER_GROUP = N_EXPERTS // N_EXPERT_GROUPS
TOP_K = 2
D_EXPERT = 512
N_EVEN = (DEPTH + 1) // 2
N_ODD = DEPTH // 2
EVEN_SPLITS = (GLA_HEADS * GLA_DK, GLA_HEADS * GLA_DK, GLA_HEADS * GLA_DV, GLA_GATE_RANK,
               GLA_HEADS * GLA_DV, SSD_INNER, SSD_CONV_DIM, SSD_HEADS)
EVEN_IN = sum(EVEN_SPLITS)
EVEN_MIX = GLA_HEADS * GLA_DV + SSD_INNER
ODD_SPLITS = (LRU_WIDTH, LRU_WIDTH, MLA_Q_LORA, MLA_KV_LORA, MLA_ROPE)
ODD_IN = sum(ODD_SPLITS)
ODD_MIX = LRU_WIDTH + MLA_HEADS * MLA_V

kernel_name = 'hybrid_gla_ssd_rglru_mla_moe'


def _split(u, sizes):
    offs, acc = [], 0
    for s in sizes[:-1]:
        acc += s
        offs.append(acc)
    return jnp.split(u, offs, axis=-1)


def rms_norm(x, g):
    xf = x.astype(jnp.float32)
    y = xf * lax.rsqrt(jnp.mean(xf * xf, axis=-1, keepdims=True) + EPS)
    return (y * g).astype(x.dtype)


def modulate(x, g, shift, scale):
    return rms_norm(x, g) * (1.0 + scale) + shift


def causal_dwconv(x, w, b):
    ch = x.shape[-1]
    y = lax.conv_general_dilated(x, w.astype(x.dtype)[:, None, :], window_strides=(1,),
                                 padding=[(CONV_K - 1, 0)],
                                 dimension_numbers=('NWC', 'WIO', 'NWC'),
                                 feature_group_count=ch)
    return y + b.astype(x.dtype)


def gla_chunked(q, k, v, log_a):
    b_, s_, h_, dk = q.shape
    dv = v.shape[-1]
    nc = s_ // CHUNK
    q = (q * dk ** -0.5).reshape(b_, nc, CHUNK, h_, dk)
    k = k.reshape(b_, nc, CHUNK, h_, dk)
    v = v.reshape(b_, nc, CHUNK, h_, dv)
    cum = jnp.cumsum(log_a.reshape(b_, nc, CHUNK, h_, dk), axis=2)
    last = cum[:, :, -1:]
    q_dec = q * jnp.exp(cum)
    k_inv = k * jnp.exp(-cum)
    k_end = k * jnp.exp(last - cum)
    causal = jnp.tril(jnp.ones((CHUNK, CHUNK), dtype=bool))
    att = jnp.where(causal, jnp.einsum('bnihd,bnjhd->bnhij', q_dec, k_inv), 0.0)
    o_intra = jnp.einsum('bnhij,bnjhe->bnihe', att, v)
    upd = jnp.einsum('bnjhd,bnjhe->nbhde', k_end, v)
    decay = jnp.moveaxis(jnp.exp(last[:, :, 0]), 1, 0)
    q_scan = jnp.moveaxis(q_dec, 1, 0)

    def step(state, xs):
        upd_c, dec_c, q_c = xs
        o_c = jnp.einsum('bihd,bhde->bihe', q_c, state)
        return dec_c[..., None] * state + upd_c, o_c

    init = jnp.zeros((b_, h_, dk, dv), q.dtype)
    _, o_inter = lax.scan(step, init, (upd, decay, q_scan))
    o = o_intra + jnp.moveaxis(o_inter, 0, 1)
    return o.reshape(b_, s_, h_, dv)


def ssd_chunked(x, dt, a, bm, cm):
    b_, s_, g_, hg, p_ = x.shape
    n_ = bm.shape[-1]
    nc = s_ // CHUNK
    x = x.reshape(b_, nc, CHUNK, g_, hg, p_)
    dt = dt.reshape(b_, nc, CHUNK, g_, hg)
    bm = bm.reshape(b_, nc, CHUNK, g_, n_)
    cm = cm.reshape(b_, nc, CHUNK, g_, n_)
    cum = jnp.cumsum(dt * a, axis=2)
    causal = jnp.tril(jnp.ones((CHUNK, CHUNK), dtype=bool))[:, :, None, None]
    seg = cum[:, :, :, None] - cum[:, :, None, :]
    decay_in = jnp.exp(jnp.where(causal, seg, -jnp.inf))
    cb = jnp.einsum('bclgn,bcsgn->bclsg', cm, bm)
    w = cb[..., None] * decay_in * dt[:, :, None]
    y_diag = jnp.einsum('bclsgh,bcsghp->bclghp', w, x)
    last = cum[:, :, -1:]
    states = jnp.einsum('bclgn,bclgh,bclghp->cbghpn', bm, jnp.exp(last - cum) * dt, x)
    chunk_decay = jnp.moveaxis(jnp.exp(last[:, :, 0]), 1, 0)
    c_scan = jnp.moveaxis(cm, 1, 0)
    in_decay = jnp.moveaxis(jnp.exp(cum), 1, 0)

    def step(h, xs):
        st_c, dec_c, c_c, e_c = xs
        y_c = jnp.einsum('blgn,bghpn,blgh->blghp', c_c, h, e_c)
        return dec_c[..., None, None] * h + st_c, y_c

    init = jnp.zeros((b_, g_, hg, p_, n_), x.dtype)
    _, y_off = lax.scan(step, init, (states, chunk_decay, c_scan, in_decay))
    y = y_diag + jnp.moveaxis(y_off, 0, 1)
    return y.reshape(b_, s_, g_, hg, p_)


def rg_lru(x, w_a, b_a, w_x, b_x, lam):
    b_, s_, w_ = x.shape
    xb = x.reshape(b_, s_, LRU_BLOCKS, LRU_BLOCK_W)
    r = jax.nn.sigmoid(jnp.einsum('bsnw,nwv->bsnv', xb, w_a).reshape(b_, s_, w_) + b_a)
    i = jax.nn.sigmoid(jnp.einsum('bsnw,nwv->bsnv', xb, w_x).reshape(b_, s_, w_) + b_x)
    log_a = -LRU_C * r * jax.nn.softplus(-lam.astype(jnp.float32))
    a = jnp.exp(log_a)
    u = jnp.sqrt(-jnp.expm1(2.0 * log_a)) * (i * x)

    def combine(left, right):
        a_l, u_l = left
        a_r, u_r = right
        return a_l * a_r, a_r * u_l + u_r

    _, h = lax.associative_scan(combine, (a, u), axis=1)
    return h


def rope(x, cos, sin):
    x1, x2 = jnp.split(x, 2, axis=-1)
    return jnp.concatenate([x1 * cos - x2 * sin, x1 * sin + x2 * cos], axis=-1)


def blocked_causal_attention(q, k, v):
    s_ = q.shape[1]
    scale = q.shape[-1] ** -0.5
    outs = []
    for blk in range(s_ // Q_BLOCK):
        q0, kv_len = blk * Q_BLOCK, (blk + 1) * Q_BLOCK
        sc = jnp.einsum('bqhd,bkhd->bhqk', q[:, q0:kv_len], k[:, :kv_len]).astype(jnp.float32) * scale
        mask = jnp.arange(kv_len)[None, :] <= (q0 + jnp.arange(Q_BLOCK))[:, None]
        p = jax.nn.softmax(jnp.where(mask, sc, -jnp.inf), axis=-1)
        outs.append(jnp.einsum('bhqk,bkhd->bqhd', p.astype(v.dtype), v[:, :kv_len]))
    return jnp.concatenate(outs, axis=1)


def even_mixer(h, w_in, gla_w_g2, gla_b_g2, gla_onorm, ssd_conv_w, ssd_conv_b,
               ssd_dt_bias, ssd_a_log, ssd_d, ssd_norm, w_out):
    f32 = jnp.float32
    b_, s_, _ = h.shape
    u = h @ w_in
    q, k, v, g_lr, og, z, xbc, dt = _split(u, EVEN_SPLITS)
    log_a = jax.nn.log_sigmoid((g_lr @ gla_w_g2 + gla_b_g2).astype(f32)) / GLA_GATE_NORM
    hd = lambda t, d: t.astype(f32).reshape(b_, s_, GLA_HEADS, d)
    o_gla = gla_chunked(hd(q, GLA_DK), hd(k, GLA_DK), hd(v, GLA_DV), hd(log_a, GLA_DK))
    o_gla = rms_norm(o_gla, gla_onorm.reshape(GLA_HEADS, GLA_DV)).reshape(b_, s_, -1)
    o_gla = o_gla.astype(h.dtype) * jax.nn.silu(og)
    xbc = jax.nn.silu(causal_dwconv(xbc, ssd_conv_w, ssd_conv_b))
    xs, bm, cm = _split(xbc, (SSD_INNER, SSD_GROUPS * SSD_STATE, SSD_GROUPS * SSD_STATE))
    dt = jax.nn.softplus(dt.astype(f32) + ssd_dt_bias)
    a = -jnp.exp(ssd_a_log.astype(f32))
    xs_h = xs.astype(f32).reshape(b_, s_, SSD_GROUPS, SSD_HPG, SSD_HEADDIM)
    y = ssd_chunked(xs_h, dt.reshape(b_, s_, SSD_GROUPS, SSD_HPG), a.reshape(SSD_GROUPS, SSD_HPG),
                    bm.astype(f32).reshape(b_, s_, SSD_GROUPS, SSD_STATE),
                    cm.astype(f32).reshape(b_, s_, SSD_GROUPS, SSD_STATE))
    y = y + ssd_d.reshape(SSD_GROUPS, SSD_HPG)[..., None] * xs_h
    y = y.reshape(b_, s_, SSD_INNER) * jax.nn.silu(z.astype(f32))
    y = rms_norm(y.reshape(b_, s_, SSD_GROUPS, SSD_INNER // SSD_GROUPS),
                 ssd_norm.reshape(SSD_GROUPS, -1)).reshape(b_, s_, SSD_INNER)
    mix = jnp.concatenate([o_gla, y.astype(h.dtype)], axis=-1)
    return mix @ w_out


def odd_mixer(h, positions, w_in, lru_conv_w, lru_conv_b, lru_w_a, lru_b_a, lru_w_x, lru_b_x,
              lru_lambda, mla_q_norm, mla_w_q_up, mla_kv_norm, mla_w_kv_up, mla_q_qknorm,
              mla_k_qknorm, w_out):
    f32 = jnp.float32
    b_, s_, _ = h.shape
    u = h @ w_in
    gate, xr, u_q, u_kv, k_r = _split(u, ODD_SPLITS)
    xr = causal_dwconv(xr, lru_conv_w, lru_conv_b)
    hr = rg_lru(xr.astype(f32), lru_w_a, lru_b_a, lru_w_x, lru_b_x, lru_lambda)
    o_lru = hr.astype(h.dtype) * jax.nn.gelu(gate)
    q = (rms_norm(u_q, mla_q_norm) @ mla_w_q_up).reshape(b_, s_, MLA_HEADS, MLA_QK_DIM)
    kv = (rms_norm(u_kv, mla_kv_norm) @ mla_w_kv_up).reshape(b_, s_, MLA_HEADS, MLA_NOPE + MLA_V)
    k_nope, v = jnp.split(kv, [MLA_NOPE], axis=-1)
    k = jnp.concatenate([k_nope, jnp.broadcast_to(k_r[:, :, None, :], (b_, s_, MLA_HEADS, MLA_ROPE))], axis=-1)
    q = rms_norm(q, mla_q_qknorm)
    k = rms_norm(k, mla_k_qknorm)
    freqs = ROPE_THETA ** (-jnp.arange(0, MLA_ROPE, 2, dtype=f32) / MLA_ROPE)
    ang = positions.astype(f32)[..., None] * freqs
    cos = jnp.cos(ang)[:, :, None, :].astype(q.dtype)
    sin = jnp.sin(ang)[:, :, None, :].astype(q.dtype)
    q = jnp.concatenate([q[..., :MLA_NOPE], rope(q[..., MLA_NOPE:], cos, sin)], axis=-1)
    k = jnp.concatenate([k[..., :MLA_NOPE], rope(k[..., MLA_NOPE:], cos, sin)], axis=-1)
    o_mla = blocked_causal_attention(q, k, v).reshape(b_, s_, MLA_HEADS * MLA_V)
    mix = jnp.concatenate([o_lru, o_mla], axis=-1)
    return mix @ w_out


def moe(h, router_w, router_bias, w_gate, w_up, w_down):
    f32 = jnp.float32
    b_, s_, d_ = h.shape
    xt = h.reshape(-1, d_)
    scores = jax.nn.sigmoid(xt.astype(f32) @ router_w.astype(f32))
    sel = scores + router_bias.astype(f32)
    grp = sel.reshape(-1, N_EXPERT_GROUPS, EXPERTS_PER_GROUP)
    grp_score = lax.top_k(grp, 2)[0].sum(axis=-1)
    best = jnp.argmax(grp_score, axis=-1)
    in_group = (jnp.arange(N_EXPERTS) // EXPERTS_PER_GROUP)[None, :] == best[:, None]
    _, idx = lax.top_k(jnp.where(in_group, sel, -jnp.inf), TOP_K)
    wts = jnp.take_along_axis(scores, idx, axis=-1)
    wts = wts / jnp.sum(wts, axis=-1, keepdims=True)
    combine = jnp.einsum('tk,tke->te', wts, jax.nn.one_hot(idx, N_EXPERTS, dtype=f32))
    out = jnp.zeros_like(xt)
    for e in range(N_EXPERTS):
        hid = jax.nn.silu(xt @ w_gate[e]) * (xt @ w_up[e])
        out = out + combine[:, e:e + 1].astype(xt.dtype) * (hid @ w_down[e])
    return out.reshape(b_, s_, d_)


def setup_inputs(seed: int = 0) -> dict:
    key = jax.random.key(seed)
    ks = iter(jax.random.split(key, 64))
    f32 = jnp.float32

    def nrm(shape, scale):
        return scale * jax.random.normal(next(ks), shape, f32)

    def gain(shape):
        return 1.0 + 0.05 * jax.random.normal(next(ks), shape, f32)

    def unif(shape, lo, hi):
        return jax.random.uniform(next(ks), shape, f32, lo, hi)

    D = D_MODEL
    x = nrm((BATCH, SEQ, D), 1.0)
    c = nrm((BATCH, D), 1.0)
    offsets = jax.random.randint(next(ks), (BATCH, 1), 0, 1024, dtype=jnp.int32)
    positions = offsets + jnp.arange(SEQ, dtype=jnp.int32)[None, :]
    router_w = nrm((D, N_EXPERTS), D ** -0.5)
    router_bias = nrm((N_EXPERTS,), 0.01)
    ada_w = nrm((DEPTH, D, 6 * D), 0.5 * D ** -0.5)
    ada_b = nrm((DEPTH, 6 * D), 0.02)
    norm_mix = gain((DEPTH, D))
    norm_ffn = gain((DEPTH, D))
    moe_w_gate = nrm((DEPTH, N_EXPERTS, D, D_EXPERT), D ** -0.5)
    moe_w_up = nrm((DEPTH, N_EXPERTS, D, D_EXPERT), D ** -0.5)
    moe_w_down = nrm((DEPTH, N_EXPERTS, D_EXPERT, D), D_EXPERT ** -0.5)
    ev_w_in = nrm((N_EVEN, D, EVEN_IN), D ** -0.5)
    gla_w_g2 = nrm((N_EVEN, GLA_GATE_RANK, GLA_HEADS * GLA_DK), GLA_GATE_RANK ** -0.5)
    gla_b_g2 = nrm((N_EVEN, GLA_HEADS * GLA_DK), 0.1)
    gla_onorm = gain((N_EVEN, GLA_HEADS * GLA_DV))
    ssd_conv_w = nrm((N_EVEN, CONV_K, SSD_CONV_DIM), CONV_K ** -0.5)
    ssd_conv_b = nrm((N_EVEN, SSD_CONV_DIM), 0.02)
    dt0 = jnp.exp(unif((N_EVEN, SSD_HEADS), math.log(1e-3), math.log(1e-1)))
    ssd_dt_bias = dt0 + jnp.log(-jnp.expm1(-dt0))
    ssd_a_log = jnp.log(unif((N_EVEN, SSD_HEADS), 1.0, 16.0))
    ssd_d = gain((N_EVEN, SSD_HEADS))
    ssd_norm = gain((N_EVEN, SSD_INNER))
    ev_w_out = nrm((N_EVEN, EVEN_MIX, D), EVEN_MIX ** -0.5)
    od_w_in = nrm((N_ODD, D, ODD_IN), D ** -0.5)
    lru_conv_w = nrm((N_ODD, CONV_K, LRU_WIDTH), CONV_K ** -0.5)
    lru_conv_b = nrm((N_ODD, LRU_WIDTH), 0.02)
    lru_w_a = nrm((N_ODD, LRU_BLOCKS, LRU_BLOCK_W, LRU_BLOCK_W), LRU_BLOCK_W ** -0.5)
    lru_b_a = nrm((N_ODD, LRU_WIDTH), 0.1)
    lru_w_x = nrm((N_ODD, LRU_BLOCKS, LRU_BLOCK_W, LRU_BLOCK_W), LRU_BLOCK_W ** -0.5)
    lru_b_x = nrm((N_ODD, LRU_WIDTH), 0.1)
    a0 = unif((N_ODD, LRU_WIDTH), 0.9, 0.999) ** (1.0 / LRU_C)
    lru_lambda = jnp.log(a0) - jnp.log1p(-a0)
    mla_q_norm = gain((N_ODD, MLA_Q_LORA))
    mla_w_q_up = nrm((N_ODD, MLA_Q_LORA, MLA_HEADS * MLA_QK_DIM), MLA_Q_LORA ** -0.5)
    mla_kv_norm = gain((N_ODD, MLA_KV_LORA))
    mla_w_kv_up = nrm((N_ODD, MLA_KV_LORA, MLA_HEADS * (MLA_NOPE + MLA_V)), MLA_KV_LORA ** -0.5)
    mla_q_qknorm = gain((N_ODD, MLA_QK_DIM))
    mla_k_qknorm = gain((N_ODD, MLA_QK_DIM))
    od_w_out = nrm((N_ODD, ODD_MIX, D), ODD_MIX ** -0.5)
    return {'x': x, 'c': c, 'positions': positions, 'router_w': router_w, 'router_bias': router_bias,
            'ada_w': ada_w, 'ada_b': ada_b, 'norm_mix': norm_mix, 'norm_ffn': norm_ffn,
            'moe_w_gate': moe_w_gate, 'moe_w_up': moe_w_up, 'moe_w_down': moe_w_down,
            'ev_w_in': ev_w_in, 'gla_w_g2': gla_w_g2, 'gla_b_g2': gla_b_g2, 'gla_onorm': gla_onorm,
            'ssd_conv_w': ssd_conv_w, 'ssd_conv_b': ssd_conv_b, 'ssd_dt_bias': ssd_dt_bias,
            'ssd_a_log': ssd_a_log, 'ssd_d': ssd_d, 'ssd_norm': ssd_norm, 'ev_w_out': ev_w_out,
            'od_w_in': od_w_in, 'lru_conv_w': lru_conv_w, 'lru_conv_b': lru_conv_b,
            'lru_w_a': lru_w_a, 'lru_b_a': lru_b_a, 'lru_w_x': lru_w_x, 'lru_b_x': lru_b_x,
            'lru_lambda': lru_lambda, 'mla_q_norm': mla_q_norm, 'mla_w_q_up': mla_w_q_up,
            'mla_kv_norm': mla_kv_norm, 'mla_w_kv_up': mla_w_kv_up, 'mla_q_qknorm': mla_q_qknorm,
            'mla_k_qknorm': mla_k_qknorm, 'od_w_out': od_w_out}


def reference(x, c, positions, router_w, router_bias, ada_w, ada_b, norm_mix, norm_ffn,
              moe_w_gate, moe_w_up, moe_w_down, ev_w_in, gla_w_g2, gla_b_g2, gla_onorm,
              ssd_conv_w, ssd_conv_b, ssd_dt_bias, ssd_a_log, ssd_d, ssd_norm, ev_w_out,
              od_w_in, lru_conv_w, lru_conv_b, lru_w_a, lru_b_a, lru_w_x, lru_b_x, lru_lambda,
              mla_q_norm, mla_w_q_up, mla_kv_norm, mla_w_kv_up, mla_q_qknorm, mla_k_qknorm,
              od_w_out):
    h = x
    c_act = jax.nn.silu(c)
    for layer in range(DEPTH):
        mod = (c_act @ ada_w[layer] + ada_b[layer])[:, None, :]
        sh1, sc1, g1, sh2, sc2, g2 = jnp.split(mod, 6, axis=-1)
        hm = modulate(h, norm_mix[layer], sh1, sc1)
        i = layer // 2
        if layer % 2 == 0:
            y = even_mixer(hm, ev_w_in[i], gla_w_g2[i], gla_b_g2[i], gla_onorm[i], ssd_conv_w[i],
                           ssd_conv_b[i], ssd_dt_bias[i], ssd_a_log[i], ssd_d[i], ssd_norm[i],
                           ev_w_out[i])
        else:
            y = odd_mixer(hm, positions, od_w_in[i], lru_conv_w[i], lru_conv_b[i], lru_w_a[i],
                          lru_b_a[i], lru_w_x[i], lru_b_x[i], lru_lambda[i], mla_q_norm[i],
                          mla_w_q_up[i], mla_kv_norm[i], mla_w_kv_up[i], mla_q_qknorm[i],
                          mla_k_qknorm[i], od_w_out[i])
        h = h + g1 * y
        hf = modulate(h, norm_ffn[layer], sh2, sc2)
        h = h + g2 * moe(hf, router_w, router_bias, moe_w_gate[layer], moe_w_up[layer],
                         moe_w_down[layer])
    return h
```

```python
import re
import math
import numpy as np
import ml_dtypes
from contextlib import ExitStack
import concourse.bass as bass
import concourse.mybir as mybir
from concourse.bass_utils import run_bass_kernel_spmd

F32 = mybir.dt.float32
BF16 = mybir.dt.bfloat16
I32 = mybir.dt.int32
AF = mybir.ActivationFunctionType
ALU = mybir.AluOpType
AX = mybir.AxisListType

ENGS = ["sp", "act", "dve", "pool", "pe"]
EPS = 1e-6
NEG = -30000.0


class Buf:
    def __init__(self, t, name=""):
        self.t = t
        self.name = name
        self.w = {}
        self.r = {}
        self.dkey = None
        self.excl = False

    def __getitem__(self, idx):
        return self.t[idx]


class Prog:
    def __init__(self, nc):
        self.nc = nc
        self.phase = -1
        self.es = None

    def begin(self):
        self.phase += 1
        self.es = ExitStack()
        self.es.enter_context(self.nc.cleanup_on_exit())
        self.ops = {e: [] for e in ENGS}
        self.cnt = {}
        self.seen = {e: {} for e in ENGS}
        self.sems = {}
        self.psum_i = 0
        self.psum_b = None

    def _sem(self, key):
        if key not in self.sems:
            nm = "s%d_%s" % (self.phase, re.sub(r"[^A-Za-z0-9_]", "", str(key)))
            self.sems[key] = self.nc.alloc_semaphore(name=nm)
            self.cnt[key] = 0
        return self.sems[key]

    def sb(self, name, shape, dtype, dma=False):
        name = "p%d_%s" % (self.phase, name)
        t = self.es.enter_context(self.nc.sbuf_tensor(name, list(shape), dtype))
        b = Buf(t, name)
        if dma:
            b.dkey = ("d", name)
            self._sem(b.dkey)
        return b

    def ps(self, name, shape, dtype=F32):
        name = "p%d_%s" % (self.phase, name)
        t = self.es.enter_context(self.nc.psum_tensor(name, list(shape), dtype))
        b = Buf(t, name)
        b.excl = True
        return b

    def psum(self):
        if self.psum_b is None:
            self.psum_b = [self.ps("bank%d" % i, [128, 512], F32) for i in range(8)]
        b = self.psum_b[self.psum_i % 8]
        self.psum_i += 1
        return b

    def _collect(self, eng, reads, writes):
        need = {}
        for b in reads:
            for k, v in b.w.items():
                need[k] = max(need.get(k, 0), v)
            if b.excl:
                for k, v in b.r.items():
                    if k != eng:
                        need[k] = max(need.get(k, 0), v)
        for b in writes:
            for k, v in b.w.items():
                need[k] = max(need.get(k, 0), v)
            for k, v in b.r.items():
                need[k] = max(need.get(k, 0), v)
        waits = []
        seen = self.seen[eng]
        for k, v in need.items():
            if k[0] == "d":
                v = self.cnt[k]
            elif k == eng and (v > self.cnt[k] or eng == "pe"):
                continue
            if seen.get(k, 0) < v:
                seen[k] = v
                waits.append((k, v))
        return waits

    def op(self, eng, fn, r=(), w=(), inc=True):
        waits = self._collect(eng, r, w)
        self._sem(eng)
        tokv = self.cnt[eng] + 1
        if inc:
            self.cnt[eng] = tokv
        for b in r:
            b.r[eng] = max(b.r.get(eng, 0), tokv)
        for b in w:
            b.w[eng] = max(b.w.get(eng, 0), tokv)
        self.ops[eng].append((waits, fn, (eng, 1) if inc else None))

    def e(self, eng, meth, w=(), r=(), inc=True, **kw):
        self.op(eng, lambda E, meth=meth, kw=kw: getattr(E, meth)(**kw), r=r, w=w, inc=inc)

    def mm(self, w, out, r, lhsT, rhs, start=True, stop=True, inc=True):
        self.op("pe", lambda E: E.matmul(out, lhsT, rhs, start=start, stop=stop),
                r=r, w=[w], inc=inc)

    def tr(self, w, out, r, in_, ident, inc=True):
        self.op("pe", lambda E: E.transpose(out, in_, ident), r=r, w=[w], inc=inc)

    def dma(self, q, out_ap, in_ap, r=(), w=(), key=None, **kw):
        if key is None:
            for b in list(w) + list(r):
                if b.dkey is not None:
                    key = b.dkey
                    break
        assert key is not None, "dma needs a dma-sem buffer"
        waits = self._collect(q, r, w)
        self._sem(key)
        self.cnt[key] += 16
        tokv = self.cnt[key]
        for b in r:
            b.r[key] = max(b.r.get(key, 0), tokv)
        for b in w:
            b.w[key] = max(b.w.get(key, 0), tokv)

        def fn(E, out_ap=out_ap, in_ap=in_ap, kw=kw):
            return E.dma_start(out=out_ap, in_=in_ap, **kw)

        self.ops[q].append((waits, fn, (key, 16)))

    def end(self):
        nc = self.nc
        fin = [(k, self.cnt[k]) for k in self.sems if k[0] == "d" and self.cnt[k] > 0]
        ops = self.ops
        sems = self.sems
        with nc.Block() as block:

            def replay(E, name, extra=()):
                for waits, fn, inc in ops[name]:
                    for k, v in waits:
                        E.wait_ge(sems[k], v)
                    ins = fn(E)
                    if inc is not None:
                        ins.then_inc(sems[inc[0]], inc[1])
                for k, v in extra:
                    E.wait_ge(sems[k], v)

            @block.sync
            def _(E):
                replay(E, "sp", fin)

            @block.scalar
            def _(E):
                replay(E, "act")

            @block.vector
            def _(E):
                replay(E, "dve")

            @block.gpsimd
            def _(E):
                replay(E, "pool")

            @block.tensor
            def _(E):
                replay(E, "pe")

        self.es.close()
        self.es = None


class RPool:
    def __init__(self, P, name, n, shape, dtype, dma=False):
        self.b = [P.sb("%s%d" % (name, i), shape, dtype, dma=dma) for i in range(n)]
        self.i = 0

    def get(self):
        b = self.b[self.i % len(self.b)]
        self.i += 1
        return b


def load_const(P, name, ap, shape, dtype, q="sp"):
    b = P.sb(name, shape, dtype, dma=True)
    P.dma(q, b[:], ap, w=[b])
    return b


def load_w_bf16(P, dst, w_ap, K, N, stage, eng="pool"):
    KC = K // 128
    wv = w_ap.rearrange("(c p) n -> p c n", p=128)
    for kc in range(KC):
        for n0 in range(0, N, 1024):
            n1 = min(N, n0 + 1024)
            st = stage.get()
            P.dma("sp", st[:, 0:n1 - n0], wv[:, kc, n0:n1], w=[st])
            P.e(eng, "tensor_copy", w=[dst], r=[st], out=dst[:, kc, n0:n1], in_=st[:, 0:n1 - n0])


def rstd_of(P, ss, n, small, eng_r="dve"):
    sd = small.get()
    P.e("act", "activation", w=[sd], r=[ss], out=sd[:, 0:1], in_=ss[:, 0:1], func=AF.Sqrt,
        scale=1.0 / n, bias=EPS)
    rs = small.get()
    P.e("dve", "reciprocal", w=[rs], r=[sd], out=rs[:, 0:1], in_=sd[:, 0:1])
    return rs


def phase_ada(P, D, L):
    P.begin()
    ccol = load_const(P, "ccol", D["c_col"], [128, 8], F32)
    nmc = load_const(P, "nmc", D["nm_col%d" % L], [128, 8], F32)
    nfc = load_const(P, "nfc", D["nf_col%d" % L], [128, 8], F32)
    identf = load_const(P, "identf", D["ident_f"], [128, 128], F32)
    adab = load_const(P, "adab", D["ada_b%d" % L].to_broadcast([128, 6144]), [128, 6144], F32)
    cact = P.sb("cact", [128, 8], F32)
    P.e("act", "activation", w=[cact], r=[ccol], out=cact[:], in_=ccol[:], func=AF.Silu)
    cbc = P.sb("cbc", [128, 8, 128], F32)
    P.e("dve", "tensor_copy", w=[cbc], r=[cact], out=cbc[:],
        in_=cact[:].unsqueeze(2).to_broadcast([128, 8, 128]))
    modbc = P.sb("modbc", [128, 6144], F32, dma=True)
    stage = RPool(P, "adst", 3, [128, 3072], F32, dma=True)
    wv = D["ada_w%d" % L].rearrange("(c p) n -> p c n", p=128)
    for half in range(2):
        banks = [P.psum() for _ in range(6)]
        for kc in range(8):
            st = stage.get()
            P.dma("sp", st[:], wv[:, kc, half * 3072:(half + 1) * 3072], w=[st])
            for j in range(6):
                P.mm(banks[j], banks[j][:, :], [cbc, st], cbc[:, kc, :], st[:, j * 512:(j + 1) * 512],
                     start=(kc == 0), stop=(kc == 7))
        for j in range(6):
            c0 = half * 3072 + j * 512
            P.e("dve", "tensor_tensor", w=[modbc], r=[banks[j], adab], out=modbc[:, c0:c0 + 512],
                in0=banks[j][:, :], in1=adab[:, c0:c0 + 512], op=ALU.add)
    P.dma("sp", D["modbc%d" % L], modbc[:], r=[modbc])
    modcol = P.sb("modcol", [128, 32], F32, dma=True)
    for j, seg in enumerate((1, 0, 4, 3)):
        for half in range(2):
            tp = P.psum()
            for c4 in range(4):
                c = half * 4 + c4
                P.tr(tp, tp[:, c4 * 128:(c4 + 1) * 128], [modbc, identf],
                     modbc[:, seg * 1024 + c * 128: seg * 1024 + (c + 1) * 128], identf[:], inc=(c4 == 3))
            for c4 in range(4):
                c = half * 4 + c4
                P.e("dve", "tensor_copy", w=[modcol], r=[tp], out=modcol[:, j * 8 + c: j * 8 + c + 1],
                    in_=tp[:, c4 * 128: c4 * 128 + 1])
    for j, nb in ((0, nmc), (2, nfc)):
        P.e("dve", "scalar_tensor_tensor", w=[modcol], r=[modcol, nb], out=modcol[:, j * 8:(j + 1) * 8],
            in0=modcol[:, j * 8:(j + 1) * 8], scalar=1.0, in1=nb[:], op0=ALU.add, op1=ALU.mult)
    P.dma("sp", D["modcol%d" % L], modcol[:], r=[modcol])
    P.end()


def modulate_tile(P, xt, modcol, joff, identf, hmT, tcol, pools, out_f32=None, f32_tcol=0):
    junk, small, xnp = pools
    jk = junk.get()
    ss = small.get()
    P.e("act", "activation", w=[jk, ss], r=[xt], out=jk[:], in_=xt[:], func=AF.Square, accum_out=ss[:, 0:1])
    rs = rstd_of(P, ss, 1024, small)
    xn = xnp.get()
    P.e("dve", "tensor_scalar", w=[xn], r=[xt, rs], out=xn[:], in0=xt[:], scalar1=rs[:, 0:1], scalar2=None,
        op0=ALU.mult)
    for half in range(2):
        tp = P.psum()
        for c4 in range(4):
            c = half * 4 + c4
            P.tr(tp, tp[:, c4 * 128:(c4 + 1) * 128], [xn, identf], xn[:, c * 128:(c + 1) * 128], identf[:],
                 inc=(c4 == 3))
        for c4 in range(4):
            c = half * 4 + c4
            wcol = modcol[:, joff * 8 + c: joff * 8 + c + 1]
            scol = modcol[:, (joff + 1) * 8 + c: (joff + 1) * 8 + c + 1]
            if half == 0:
                P.e("dve", "tensor_scalar", w=[hmT], r=[tp, modcol], out=hmT[:, c, tcol:tcol + 128],
                    in0=tp[:, c4 * 128:(c4 + 1) * 128], scalar1=wcol, scalar2=scol, op0=ALU.mult, op1=ALU.add)
            else:
                P.e("act", "activation", w=[hmT], r=[tp, modcol], out=hmT[:, c, tcol:tcol + 128],
                    in_=tp[:, c4 * 128:(c4 + 1) * 128], func=AF.Identity, scale=wcol, bias=scol)
            if out_f32 is not None:
                P.e("dve", "tensor_scalar", w=[out_f32], r=[tp, modcol], out=out_f32[:, c, f32_tcol:f32_tcol + 128],
                    in0=tp[:, c4 * 128:(c4 + 1) * 128], scalar1=wcol, scalar2=scol, op0=ALU.mult, op1=ALU.add)


def phase_m0(P, D, S_TOK=8192):
    LVL, SUB, VAR = 9, 9, 'ABCD'
    P.begin()
    NST = S_TOK // 512
    identf = load_const(P, "identf", D["ident_f"], [128, 128], F32)
    identb = load_const(P, "identb", D["ident_b"], [128, 128], BF16)
    triu = load_const(P, "triu", D["triu"], [128, 128], F32)
    maskneg = load_const(P, "maskneg", D["maskneg"], [128, 512], F32)
    eye8 = load_const(P, "eye8", D["eye8"], [8, 8], F32)
    ones8 = load_const(P, "ones8", D["ones8"], [8, 128], F32)
    rm128 = load_const(P, "rm128", D["rm128"], [128, 512], F32)
    modcol = load_const(P, "modcol", D["modcol0"], [128, 32], F32)
    bg2n = load_const(P, "bg2n", D["bg2_col"], [128, 1], F32)
    P.e("dve", "tensor_scalar", w=[bg2n], r=[bg2n], out=bg2n[:], in0=bg2n[:], scalar1=-1.0, scalar2=None,
        op0=ALU.mult)
    onorm = load_const(P, "onorm", D["onorm_row"].to_broadcast([128, 256]), [128, 256], F32)
    ssdn = load_const(P, "ssdn", D["ssdn_row"].to_broadcast([128, 512]), [128, 512], F32)
    convw = load_const(P, "convw", D["convw"], [128, 24], F32)
    convb = load_const(P, "convb", D["convb"], [128, 6], F32)
    dtb = load_const(P, "dtb", D["dtb_col"], [8, 1], F32)
    aneg = load_const(P, "aneg", D["alog_col"], [8, 1], F32)
    P.e("act", "activation", w=[aneg], r=[aneg], out=aneg[:], in_=aneg[:], func=AF.Exp)
    P.e("dve", "tensor_scalar", w=[aneg], r=[aneg], out=aneg[:], in0=aneg[:], scalar1=-1.0, scalar2=None,
        op0=ALU.mult)
    dsk = load_const(P, "dsk", D["dskip_row"].to_broadcast([128, 8]), [128, 8], F32)
    Dmat = P.sb("Dmat", [128, 8, 128], BF16)
    for h in range(8):
        P.e("dve", "tensor_scalar", w=[Dmat], r=[identb, dsk], out=Dmat[:, h, :], in0=identb[:],
            scalar1=dsk[:, h:h + 1], scalar2=None, op0=ALU.mult)
    stage = RPool(P, "wst", 2, [128, 1024], F32, dma=True)
    wfm = P.sb("wfm", [128, 8, 1024], BF16)
    wtm = P.sb("wtm", [128, 8, 1024], BF16)
    wglr = P.sb("wglr", [128, 8, 16], BF16)
    wdt = P.sb("wdt", [128, 8, 8], BF16)
    load_w_bf16(P, wfm, D["w_fm"], 1024, 1024, stage)
    load_w_bf16(P, wtm, D["w_tm"], 1024, 1024, stage)
    load_w_bf16(P, wglr, D["w_glr"], 1024, 16, stage)
    load_w_bf16(P, wdt, D["w_dt"], 1024, 8, stage)
    wg2f = load_const(P, "wg2f", D["w_g2"], [16, 128], F32)
    wg2 = P.sb("wg2", [16, 128], BF16)
    P.e("dve", "tensor_copy", w=[wg2], r=[wg2f], out=wg2[:], in_=wg2f[:])
    S = P.sb("S", [128, 128], F32)
    Sbf = P.sb("Sbf", [128, 128], BF16)
    HT = P.sb("HT", [128, 512], F32)
    HTbf = P.sb("HTbf", [128, 512], BF16)
    P.e("pool", "memset", w=[S], ap=S[:], constant=0.0)
    P.e("pool", "memset", w=[Sbf], ap=Sbf[:], constant=0.0)
    P.e("pool", "memset", w=[HT], ap=HT[:], constant=0.0)
    P.e("pool", "memset", w=[HTbf], ap=HTbf[:], constant=0.0)
    xbcf = [P.sb("xbcf%d" % i, [128, 515], F32) for i in range(6)]
    for b in xbcf:
        P.e("pool", "memset", w=[b], ap=b[:], constant=0.0)
    xpool = RPool(P, "xt", 2, [128, 1024], F32, dma=True)
    junk = RPool(P, "junk", 2, [128, 1024], BF16)
    small = RPool(P, "small", 24, [128, 1], F32)
    xnp = RPool(P, "xn", 1, [128, 1024], F32)
    hmp = RPool(P, "hmT", 2, [128, 8, 512], BF16)
    qkp = RPool(P, "qk", 2, [128, 512], F32)
    g512 = RPool(P, "g512", 4, [128, 512], F32)
    Eep = RPool(P, "Ee", 2, [128, 512], F32)
    accp = RPool(P, "acc", 2, [128, 512], F32)
    typ = RPool(P, "ty", 2, [128, 512], F32)
    ctmp = RPool(P, "ctmp", 2, [128, 512], F32)
    glrp = RPool(P, "glr", 2, [128, 512], BF16)
    qdkip = RPool(P, "qdki", 2, [128, 512], BF16)
    xTp = RPool(P, "xT", 6, [128, 512], BF16)
    t512b = RPool(P, "t512b", 6, [128, 512], BF16)
    vtokp = RPool(P, "vtok", 4, [128, 256], BF16)
    gwp = RPool(P, "gw", 4, [128, 256], F32)
    szp = RPool(P, "sz", 4, [128, 512], F32)
    b128 = RPool(P, "b128", 8, [128, 128], BF16)
    f128 = RPool(P, "f128", 4, [128, 128], F32)
    mixtp = RPool(P, "mixtok", 3, [128, 768], BF16)
    mixTp = RPool(P, "mixT", 2, [128, 6, 512], BF16, dma=True)
    s8p = RPool(P, "s8", 4, [8, 512], F32)
    Rp = RPool(P, "R", 1, [8, 8, 128], F32)
    segp = RPool(P, "seg", 1, [128, 8, 128], F32)
    w8p = RPool(P, "w8", 2, [128, 8, 128], BF16)
    c16p = RPool(P, "c16", 6, [128, 16], F32)
    xv = D["x"]
    mixT_d = D["mixT0"]

    for st in range(NST if LVL >= 1 else 0):
        hmT = hmp.get()
        for t in range(4):
            xt = xpool.get()
            r0 = (st * 4 + t) * 128
            P.dma("sp", xt[:], xv[r0:r0 + 128, :], w=[xt])
            modulate_tile(P, xt, modcol, 0, identf, hmT, t * 128, (junk, small, xnp))
        if SUB < 2:
            continue
        qf = qkp.get()
        kf = qkp.get()
        for g in range(8):
            pj = P.psum()
            for kc in range(8):
                P.mm(pj, pj[:, :], [wfm, hmT], wfm[:, kc, g * 128:(g + 1) * 128], hmT[:, kc, :],
                     start=(kc == 0), stop=(kc == 7))
            if g == 0:
                P.e("act", "activation", w=[qf], r=[pj], out=qf[:], in_=pj[:, :], func=AF.Copy)
            elif g == 1:
                P.e("dve", "tensor_copy", w=[kf], r=[pj], out=kf[:], in_=pj[:, :])
            else:
                xb = xbcf[g - 2]
                P.e("pool", "tensor_copy", w=[xb], r=[xb], out=xb[:, 0:3], in_=xb[:, 512:515])
                if g % 2 == 0:
                    P.e("act", "activation", w=[xb], r=[pj], out=xb[:, 3:515], in_=pj[:, :], func=AF.Copy)
                else:
                    P.e("dve", "tensor_copy", w=[xb], r=[pj], out=xb[:, 3:515], in_=pj[:, :])
        if SUB < 3:
            continue
        pg = P.psum()
        for kc in range(8):
            P.mm(pg, pg[0:16, :], [wglr, hmT], wglr[:, kc, :], hmT[:, kc, :], start=(kc == 0), stop=(kc == 7))
        glr = glrp.get()
        P.e("act", "activation", w=[glr], r=[pg], out=glr[0:16, :], in_=pg[0:16, :], func=AF.Copy)
        pd = P.psum()
        for kc in range(8):
            P.mm(pd, pd[0:8, :], [wdt, hmT], wdt[:, kc, :], hmT[:, kc, :], start=(kc == 0), stop=(kc == 7))
        edt = s8p.get()
        P.e("act", "activation", w=[edt], r=[pd, dtb], out=edt[:], in_=pd[0:8, :], func=AF.Exp, bias=dtb[:, 0:1])
        dt = s8p.get()
        P.e("act", "activation", w=[dt], r=[edt], out=dt[:], in_=edt[:], func=AF.Ln, bias=1.0)
        if SUB < 4:
            continue
        vtok, gw, sz = [], [], []
        for t in range(4):
            tc = slice(t * 128, (t + 1) * 128)
            pv = P.psum()
            for kc in range(8):
                P.mm(pv, pv[:, :], [wtm, hmT], hmT[:, kc, tc], wtm[:, kc, 0:512], start=(kc == 0), stop=(kc == 7))
            v_ = vtokp.get()
            if "H" not in VAR:
                P.e("dve", "tensor_copy", w=[v_], r=[pv], out=v_[:], in_=pv[:, 0:256])
            g_ = gwp.get()
            if "H" in VAR:
                P.e("act", "activation", w=[g_], r=[pv], out=g_[:], in_=pv[:, 256:512], func=AF.Silu)
            if "I" in VAR:
                P.e("act", "activation", w=[g_], r=[pv, v_], out=g_[:], in_=pv[:, 256:512], func=AF.Silu)
            if "B" in VAR:
                P.e("act", "activation", w=[g_], r=[pv], out=g_[:], in_=pv[:, 256:512], func=AF.Silu)
            if "E" in VAR:
                P.e("act", "activation", w=[g_], r=[pv], out=g_[:], in_=pv[:, 256:512], func=AF.Sigmoid)
            if "G" in VAR:
                P.e("act", "activation", w=[g_], r=[pv], out=g_[:], in_=pv[:, 256:512], func=AF.Copy)
                P.e("act", "activation", w=[g_], r=[g_], out=g_[:], in_=g_[:], func=AF.Silu)
            if "F" in VAR:
                P.e("dve", "tensor_copy", w=[g_], r=[pv], out=g_[:], in_=pv[:, 256:512])
                P.e("act", "activation", w=[g_], r=[g_], out=g_[:], in_=g_[:], func=AF.Silu)
            if "C" in VAR:
                P.e("pool", "tensor_tensor", w=[g_], r=[g_, onorm], out=g_[:], in0=g_[:], in1=onorm[:], op=ALU.mult)
            z_ = szp.get()
            if "D" in VAR:
                pz = P.psum()
                for kc in range(8):
                    P.mm(pz, pz[:, :], [wtm, hmT], hmT[:, kc, tc], wtm[:, kc, 512:1024], start=(kc == 0), stop=(kc == 7))
                P.e("act", "activation", w=[z_], r=[pz], out=z_[:], in_=pz[:, :], func=AF.Silu)
            vtok.append(v_)
            gw.append(g_)
            sz.append(z_)
        if SUB < 5:
            continue
        zps = P.psum()
        P.mm(zps, zps[:, :], [wg2, glr], wg2[:, :], glr[0:16, :])
        ez = g512.get()
        P.e("act", "activation", w=[ez], r=[zps, bg2n], out=ez[:], in_=zps[:, :], func=AF.Exp, scale=-1.0,
            bias=bg2n[:, 0:1])
        lz = g512.get()
        P.e("act", "activation", w=[lz], r=[ez], out=lz[:], in_=ez[:], func=AF.Ln, bias=1.0)
        cuml = g512.get()
        P.e("dve", "tensor_tensor_scan", w=[cuml], r=[rm128, lz], out=cuml[:], data0=rm128[:], data1=lz[:],
            initial=0.0, op0=ALU.mult, op1=ALU.add)
        Ee = Eep.get()
        P.e("act", "activation", w=[Ee], r=[cuml], out=Ee[:], in_=cuml[:], func=AF.Exp, scale=-1.0 / 16.0)
        Einv = g512.get()
        P.e("act", "activation", w=[Einv], r=[cuml], out=Einv[:], in_=cuml[:], func=AF.Exp, scale=1.0 / 16.0)
        qd = qdkip.get()
        P.e("dve", "scalar_tensor_tensor", w=[qd], r=[qf, Ee], out=qd[:], in0=qf[:], scalar=0.125, in1=Ee[:],
            op0=ALU.mult, op1=ALU.mult)
        ki = qdkip.get()
        P.e("dve", "tensor_tensor", w=[ki], r=[kf, Einv], out=ki[:], in0=kf[:], in1=Einv[:], op=ALU.mult)
        if SUB < 6:
            continue
        xT = []
        for ci in range(6):
            xb = xbcf[ci]
            acc = accp.get()
            P.e("pool", "tensor_scalar", w=[acc], r=[xb, convw], out=acc[:], in0=xb[:, 0:512],
                scalar1=convw[:, ci * 4:ci * 4 + 1], scalar2=None, op0=ALU.mult)
            for k in range(1, 4):
                tmpc = ctmp.get()
                P.e("pool", "tensor_scalar", w=[tmpc], r=[xb, convw], out=tmpc[:], in0=xb[:, k:k + 512],
                    scalar1=convw[:, ci * 4 + k:ci * 4 + k + 1], scalar2=None, op0=ALU.mult)
                P.e("pool", "tensor_tensor", w=[acc], r=[tmpc, acc], out=acc[:], in0=acc[:], in1=tmpc[:], op=ALU.add)
            xo = xTp.get()
            P.e("act", "activation", w=[xo], r=[acc, convb], out=xo[:], in_=acc[:], func=AF.Silu,
                bias=convb[:, ci:ci + 1])
            xT.append(xo)
        BT, CT = xT[4], xT[5]
        dta = s8p.get()
        P.e("dve", "tensor_scalar", w=[dta], r=[dt, aneg], out=dta[:], in0=dt[:], scalar1=aneg[:, 0:1], scalar2=None,
            op0=ALU.mult)
        cum = s8p.get()
        P.e("dve", "tensor_tensor_scan", w=[cum], r=[rm128, dta], out=cum[:], data0=rm128[0:8, :], data1=dta[:],
            initial=0.0, op0=ALU.mult, op1=ALU.add)
        mixT = mixTp.get()
        for t in range(4 if LVL >= 2 else 0):
            tc = slice(t * 128, (t + 1) * 128)
            mixtok = mixtp.get()
            tpb = P.psum()
            tpbv = tpb[:, :].bitcast(BF16)
            P.tr(tpb, tpbv[:, 0:128], [ki, identb], ki[:, tc], identb[:])
            ktok = b128.get()
            P.e("act", "activation", w=[ktok], r=[tpb], out=ktok[:], in_=tpbv[:, 0:128], func=AF.Copy)
            for h in range(2):
                hs = slice(h * 64, (h + 1) * 64)
                aps = P.psum()
                P.mm(aps, aps[:, 0:128], [ki, qd], ki[hs, tc], qd[hs, tc])
                att = b128.get()
                P.e("dve", "tensor_tensor", w=[att], r=[aps, triu], out=att[:], in0=aps[:, 0:128], in1=triu[:],
                    op=ALU.mult)
                ops_ = P.psum()
                P.mm(ops_, ops_[:, 0:128], [att, vtok[t]], att[:], vtok[t][:, h * 128:(h + 1) * 128],
                     start=True, stop=False)
                P.mm(ops_, ops_[:, 0:128], [qd, Sbf], qd[hs, tc], Sbf[hs, :], start=False, stop=True)
                jk = f128.get()
                ss = small.get()
                P.e("act", "activation", w=[jk, ss], r=[ops_], out=jk[:], in_=ops_[:, 0:128], func=AF.Square,
                    accum_out=ss[:, 0:1])
                rs = rstd_of(P, ss, 128, small)
                P.e("dve", "scalar_tensor_tensor", w=[mixtok], r=[ops_, rs, gw[t]],
                    out=mixtok[:, h * 128:(h + 1) * 128], in0=ops_[:, 0:128], scalar=rs[:, 0:1],
                    in1=gw[t][:, h * 128:(h + 1) * 128], op0=ALU.mult, op1=ALU.mult)
            upd = P.psum()
            P.mm(upd, upd[:, 0:256], [ktok, vtok[t]], ktok[:], vtok[t][:, :])
            tmpS = f128.get()
            for h in range(2):
                hs = slice(h * 64, (h + 1) * 64)
                P.e("dve", "tensor_tensor", w=[tmpS], r=[upd, S], out=tmpS[hs, :],
                    in0=upd[hs, h * 128:(h + 1) * 128], in1=S[hs, :], op=ALU.add)
            dcol = Ee[:, t * 128 + 127: t * 128 + 128]
            P.e("dve", "tensor_scalar", w=[S], r=[tmpS, Ee], out=S[:], in0=tmpS[:], scalar1=dcol, scalar2=None,
                op0=ALU.mult)
            P.e("act", "activation", w=[Sbf], r=[tmpS, Ee], out=Sbf[:], in_=tmpS[:], func=AF.Copy, scale=dcol)
            if LVL < 3:
                continue
            tpx = P.psum()
            tpxv = tpx[:, :].bitcast(BF16)
            for ci in range(4):
                P.tr(tpx, tpxv[:, ci * 128:(ci + 1) * 128], [xT[ci], identb], xT[ci][:, tc], identb[:],
                     inc=(ci == 3))
            xtok = t512b.get()
            P.e("act", "activation", w=[xtok], r=[tpx], out=xtok[:], in_=tpxv[:, 0:512], func=AF.Copy)
            tpB = P.psum()
            tpBv = tpB[:, :].bitcast(BF16)
            P.tr(tpB, tpBv[:, 0:128], [BT, identb], BT[:, tc], identb[:])
            Btok = b128.get()
            P.e("dve", "tensor_copy", w=[Btok], r=[tpB], out=Btok[:], in_=tpBv[:, 0:128])
            tps = P.psum()
            P.tr(tps, tps[:, 0:8], [cum, identf], cum[0:8, tc], identf[0:8, 0:8], inc=False)
            P.tr(tps, tps[:, 8:16], [dt, identf], dt[0:8, tc], identf[0:8, 0:8])
            ctok = c16p.get()
            P.e("dve", "tensor_copy", w=[ctok], r=[tps], out=ctok[:], in_=tps[:, 0:16])
            R = Rp.get()
            P.e("dve", "tensor_tensor", w=[R], r=[cum, eye8], out=R[:],
                in0=cum[0:8, tc].unsqueeze(1).to_broadcast([8, 8, 128]),
                in1=eye8[:].unsqueeze(2).to_broadcast([8, 8, 128]), op=ALU.mult)
            cb = [P.psum(), P.psum()]
            for hf in range(2):
                P.mm(cb[hf], cb[hf][:, :], [ones8, R], ones8[:], R[:, hf * 4:(hf + 1) * 4, :], start=True, stop=False)
                P.mm(cb[hf], cb[hf][:, :], [identf, maskneg], identf[:], maskneg[:], start=False, stop=True)
            seg = segp.get()
            for hf in range(2):
                P.e("dve", "tensor_tensor", w=[seg], r=[cb[hf], ctok], out=seg[:, hf * 4:(hf + 1) * 4, :],
                    in0=cb[hf][:, :].rearrange("p (h l) -> p h l", h=4),
                    in1=ctok[:, hf * 4:(hf + 1) * 4].unsqueeze(2).to_broadcast([128, 4, 128]), op=ALU.subtract)
            Dm = w8p.get()
            P.e("act", "activation", w=[Dm], r=[seg], out=Dm[:], in_=seg[:], func=AF.Exp)
            cbp = P.psum()
            P.mm(cbp, cbp[:, 0:128], [BT, CT], BT[:, tc], CT[:, tc])
            cbs = b128.get()
            P.e("act", "activation", w=[cbs], r=[cbp], out=cbs[:], in_=cbp[:, 0:128], func=AF.Copy)
            wm = w8p.get()
            P.e("dve", "tensor_tensor", w=[wm], r=[Dm, cbs], out=wm[:], in0=Dm[:],
                in1=cbs[:].unsqueeze(1).to_broadcast([128, 8, 128]), op=ALU.mult)
            xdt = t512b.get()
            P.e("dve", "tensor_tensor", w=[xdt], r=[xtok, ctok], out=xdt[:].rearrange("p (h d) -> p h d", h=8),
                in0=xtok[:].rearrange("p (h d) -> p h d", h=8),
                in1=ctok[:, 8:16].unsqueeze(2).to_broadcast([128, 8, 64]), op=ALU.mult)
            yps = P.psum()
            for h in range(8):
                P.mm(yps, yps[:, h * 64:(h + 1) * 64], [wm, xdt], wm[:, h, :], xdt[:, h * 64:(h + 1) * 64],
                     start=True, stop=False)
                P.mm(yps, yps[:, h * 64:(h + 1) * 64], [Dmat, xtok], Dmat[:, h, :], xtok[:, h * 64:(h + 1) * 64],
                     start=False, stop=True)
            yo = P.psum()
            P.mm(yo, yo[:, :], [CT, HTbf], CT[:, tc], HTbf[:])
            ed = c16p.get()
            P.e("act", "activation", w=[ed], r=[ctok], out=ed[:, 0:8], in_=ctok[:, 0:8], func=AF.Exp)
            t1 = typ.get()
            P.e("dve", "tensor_tensor", w=[t1], r=[yo, ed], out=t1[:].rearrange("p (h d) -> p h d", h=8),
                in0=yo[:, :].rearrange("p (h d) -> p h d", h=8),
                in1=ed[:, 0:8].unsqueeze(2).to_broadcast([128, 8, 64]), op=ALU.mult)
            y = typ.get()
            P.e("dve", "tensor_tensor", w=[y], r=[yps, t1], out=y[:], in0=yps[:, :], in1=t1[:], op=ALU.add)
            P.e("pool", "tensor_tensor", w=[y], r=[y, sz[t]], out=y[:], in0=y[:], in1=sz[t][:], op=ALU.mult)
            jk = junk.get()
            ss = small.get()
            P.e("act", "activation", w=[jk, ss], r=[y], out=jk[:, 0:512], in_=y[:], func=AF.Square,
                accum_out=ss[:, 0:1])
            rs = rstd_of(P, ss, 512, small)
            P.e("dve", "scalar_tensor_tensor", w=[mixtok], r=[y, rs, ssdn], out=mixtok[:, 256:768], in0=y[:],
                scalar=rs[:, 0:1], in1=ssdn[:], op0=ALU.mult, op1=ALU.mult)
            if LVL < 4:
                continue
            dl = c16p.get()
            for hf in range(2):
                lastv = cb[hf][:, :].rearrange("p (h l) -> p h l", h=4)[:, :, 127]
                P.e("dve", "tensor_tensor", w=[dl], r=[cb[hf], ctok], out=dl[:, hf * 4:(hf + 1) * 4], in0=lastv,
                    in1=ctok[:, hf * 4:(hf + 1) * 4], op=ALU.subtract)
                P.e("act", "activation", w=[dl], r=[cb[hf]], out=dl[:, 8 + hf * 4: 8 + (hf + 1) * 4], in_=lastv,
                    func=AF.Exp)
            el = c16p.get()
            P.e("act", "activation", w=[el], r=[dl], out=el[:, 0:8], in_=dl[:, 0:8], func=AF.Exp)
            xs = t512b.get()
            P.e("dve", "tensor_tensor", w=[xs], r=[xdt, el], out=xs[:].rearrange("p (h d) -> p h d", h=8),
                in0=xdt[:].rearrange("p (h d) -> p h d", h=8),
                in1=el[:, 0:8].unsqueeze(2).to_broadcast([128, 8, 64]), op=ALU.mult)
            up = P.psum()
            P.mm(up, up[:, :], [Btok, xs], Btok[:], xs[:])
            P.e("dve", "tensor_tensor", w=[HT], r=[HT, dl], out=HT[:].rearrange("p (h d) -> p h d", h=8),
                in0=HT[:].rearrange("p (h d) -> p h d", h=8),
                in1=dl[:, 8:16].unsqueeze(2).to_broadcast([128, 8, 64]), op=ALU.mult)
            P.e("dve", "tensor_tensor", w=[HT], r=[HT, up], out=HT[:], in0=HT[:], in1=up[:, :], op=ALU.add)
            P.e("pool", "tensor_copy", w=[HTbf], r=[HT], out=HTbf[:], in_=HT[:])
            if LVL < 5:
                continue
            for half in range(2):
                tpm = P.psum()
                tpmv = tpm[:, :].bitcast(BF16)
                for j in range(3):
                    c = half * 3 + j
                    P.tr(tpm, tpmv[:, j * 128:(j + 1) * 128], [mixtok, identb], mixtok[:, c * 128:(c + 1) * 128],
                         identb[:], inc=(j == 2))
                if half == 0:
                    P.e("act", "activation", w=[mixT], r=[tpm], out=mixT[:, 0:3, tc],
                        in_=tpmv[:, 0:384].rearrange("p (c t) -> p c t", c=3), func=AF.Copy)
                else:
                    P.e("dve", "tensor_copy", w=[mixT], r=[tpm], out=mixT[:, 3:6, tc],
                        in_=tpmv[:, 0:384].rearrange("p (c t) -> p c t", c=3))
        if LVL >= 5:
            P.dma("sp", mixT_d[:, :, st * 512:(st + 1) * 512].rearrange("c p t -> p c t"), mixT[:], r=[mixT])
    P.end()


def bf16(a):
    return np.asarray(a, dtype=np.float32).astype(ml_dtypes.bfloat16)


def host_consts():
    c = {}
    c["ident_f"] = np.eye(128, dtype=np.float32)
    c["ident_b"] = bf16(np.eye(128))
    j = np.arange(128)
    c["triu"] = (j[:, None] <= j[None, :]).astype(np.float32)
    mk = np.where(j[:, None] > j[None, :], NEG, 0.0).astype(np.float32)
    c["maskneg"] = np.tile(mk, (1, 4))
    c["eye8"] = np.eye(8, dtype=np.float32)
    c["ones8"] = np.ones((8, 128), dtype=np.float32)
    rm = np.ones((128, 512), dtype=np.float32)
    rm[:, ::128] = 0.0
    c["rm128"] = rm
    return c


def col128(v):
    v = np.asarray(v, dtype=np.float32).reshape(-1, 128)
    return np.ascontiguousarray(v.T)


CONST_SPECS = {"ident_f": ([128, 128], F32), "ident_b": ([128, 128], BF16), "triu": ([128, 128], F32),
               "maskneg": ([128, 512], F32), "eye8": ([8, 8], F32), "ones8": ([8, 128], F32),
               "rm128": ([128, 512], F32)}


def phase_fa(P, D, L, KCM, NTOK=4096):
    P.begin()
    NST = NTOK // 512
    identf = load_const(P, "identf", D["ident_f"], [128, 128], F32)
    modcol = load_const(P, "modcol", D["modcol%d" % L], [128, 32], F32)
    g1bc = load_const(P, "g1bc", D["modbc%d" % L][:, 2048:3072], [128, 1024], F32)
    rw = load_const(P, "rw", D["router_w"].rearrange("(c p) e -> p c e", p=128), [128, 8, 16], F32)
    rb = load_const(P, "rb", D["router_b"].to_broadcast([128, 16]), [128, 16], F32)
    stage = RPool(P, "wst", 2, [128, 1024], F32, dma=True)
    wout = P.sb("wout", [128, KCM, 1024], BF16)
    load_w_bf16(P, wout, D["w_out%d" % L], KCM * 128, 1024, stage)
    hinp = RPool(P, "hin", 2, [128, 1024], F32, dma=True)
    mixp = RPool(P, "mixT", 2, [128, KCM, 512], BF16, dma=True)
    h1p = RPool(P, "h1", 2, [128, 1024], F32, dma=True)
    tmpp = RPool(P, "tmp", 2, [128, 1024], F32)
    junk = RPool(P, "junk", 2, [128, 1024], BF16)
    small = RPool(P, "small", 16, [128, 1], F32)
    xnp = RPool(P, "xn", 1, [128, 1024], F32)
    hfp = RPool(P, "hfT", 2, [128, 8, 512], BF16, dma=True)
    hffp = RPool(P, "hfTf", 2, [128, 8, 128], F32)
    r16 = RPool(P, "r16", 16, [128, 16], F32)
    r4 = RPool(P, "r4", 8, [128, 4], F32)
    combp = RPool(P, "comb", 2, [128, 16], F32, dma=True)
    hin_d, mixT_d, hmid_d, hfT_d, comb_d = D["hin%d" % L], D["mixT%d" % L], D["hmid%d" % L], D["hfT%d" % L], D["comb%d" % L]
    BIG = 1.0e9
    for st in range(NST):
        mixT = mixp.get()
        P.dma("sp", mixT[:], mixT_d[:, :, st * 512:(st + 1) * 512].rearrange("c p t -> p c t"), w=[mixT])
        hfT = hfp.get()
        for t in range(4):
            tc = slice(t * 128, (t + 1) * 128)
            r0 = (st * 4 + t) * 128
            hin = hinp.get()
            P.dma("sp", hin[:], hin_d[r0:r0 + 128, :], w=[hin])
            y = [P.psum(), P.psum()]
            for kc in range(KCM):
                for hf in range(2):
                    P.mm(y[hf], y[hf][:, :], [mixT, wout], mixT[:, kc, tc], wout[:, kc, hf * 512:(hf + 1) * 512],
                         start=(kc == 0), stop=(kc == KCM - 1))
            tmp = tmpp.get()
            h1 = h1p.get()
            for hf in range(2):
                hs = slice(hf * 512, (hf + 1) * 512)
                P.e("dve", "tensor_tensor", w=[tmp], r=[y[hf], g1bc], out=tmp[:, hs], in0=y[hf][:, :], in1=g1bc[:, hs],
                    op=ALU.mult)
            P.e("pool", "tensor_tensor", w=[h1], r=[tmp, hin], out=h1[:], in0=tmp[:], in1=hin[:], op=ALU.add)
            P.dma("act", hmid_d[r0:r0 + 128, :], h1[:], r=[h1])
            hff = hffp.get()
            modulate_tile(P, h1, modcol, 2, identf, hfT, t * 128, (junk, small, xnp), out_f32=hff, f32_tcol=0)
            lg = P.psum()
            for kc in range(8):
                P.mm(lg, lg[:, 0:16], [hff, rw], hff[:, kc, :], rw[:, kc, :], start=(kc == 0), stop=(kc == 7))
            sc = r16.get()
            P.e("act", "activation", w=[sc], r=[lg], out=sc[:], in_=lg[:, 0:16], func=AF.Sigmoid)
            sel = r16.get()
            P.e("dve", "tensor_tensor", w=[sel], r=[sc, rb], out=sel[:], in0=sc[:], in1=rb[:], op=ALU.add)
            sel3 = sel[:].rearrange("p (g e) -> p g e", g=4)
            m1 = r4.get()
            P.e("dve", "tensor_reduce", w=[m1], r=[sel], out=m1[:], in_=sel3, axis=AX.X, op=ALU.max)
            eq1 = r16.get()
            P.e("dve", "tensor_tensor", w=[eq1], r=[sel, m1], out=eq1[:].rearrange("p (g e) -> p g e", g=4), in0=sel3,
                in1=m1[:].unsqueeze(2).to_broadcast([128, 4, 4]), op=ALU.is_equal)
            selx = r16.get()
            P.e("dve", "scalar_tensor_tensor", w=[selx], r=[eq1, sel], out=selx[:], in0=eq1[:], scalar=-BIG, in1=sel[:],
                op0=ALU.mult, op1=ALU.add)
            m2 = r4.get()
            P.e("dve", "tensor_reduce", w=[m2], r=[selx], out=m2[:], in_=selx[:].rearrange("p (g e) -> p g e", g=4),
                axis=AX.X, op=ALU.max)
            gs = r4.get()
            P.e("dve", "tensor_tensor", w=[gs], r=[m1, m2], out=gs[:], in0=m1[:], in1=m2[:], op=ALU.add)
            gmax = small.get()
            P.e("dve", "tensor_reduce", w=[gmax], r=[gs], out=gmax[:, 0:1], in_=gs[:], axis=AX.X, op=ALU.max)
            pen = r4.get()
            P.e("dve", "tensor_scalar", w=[pen], r=[gs, gmax], out=pen[:], in0=gs[:], scalar1=gmax[:, 0:1], scalar2=None,
                op0=ALU.is_equal)
            P.e("dve", "tensor_scalar", w=[pen], r=[pen], out=pen[:], in0=pen[:], scalar1=-1.0, scalar2=BIG,
                op0=ALU.add, op1=ALU.mult)
            selm = r16.get()
            P.e("dve", "tensor_tensor", w=[selm], r=[sel, pen], out=selm[:].rearrange("p (g e) -> p g e", g=4), in0=sel3,
                in1=pen[:].unsqueeze(2).to_broadcast([128, 4, 4]), op=ALU.add)
            t1 = small.get()
            P.e("dve", "tensor_reduce", w=[t1], r=[selm], out=t1[:, 0:1], in_=selm[:], axis=AX.X, op=ALU.max)
            mk1 = r16.get()
            P.e("dve", "tensor_scalar", w=[mk1], r=[selm, t1], out=mk1[:], in0=selm[:], scalar1=t1[:, 0:1], scalar2=None,
                op0=ALU.is_equal)
            selm2 = r16.get()
            P.e("dve", "scalar_tensor_tensor", w=[selm2], r=[mk1, selm], out=selm2[:], in0=mk1[:], scalar=-BIG,
                in1=selm[:], op0=ALU.mult, op1=ALU.add)
            t2 = small.get()
            P.e("dve", "tensor_reduce", w=[t2], r=[selm2], out=t2[:, 0:1], in_=selm2[:], axis=AX.X, op=ALU.max)
            mk2 = r16.get()
            P.e("dve", "tensor_scalar", w=[mk2], r=[selm2, t2], out=mk2[:], in0=selm2[:], scalar1=t2[:, 0:1], scalar2=None,
                op0=ALU.is_equal)
            P.e("dve", "tensor_tensor", w=[mk2], r=[mk1, mk2], out=mk2[:], in0=mk1[:], in1=mk2[:], op=ALU.add)
            wsc = r16.get()
            P.e("dve", "tensor_tensor", w=[wsc], r=[sc, mk2], out=wsc[:], in0=sc[:], in1=mk2[:], op=ALU.mult)
            den = small.get()
            P.e("dve", "tensor_reduce", w=[den], r=[wsc], out=den[:, 0:1], in_=wsc[:], axis=AX.X, op=ALU.add)
            rden = small.get()
            P.e("dve", "reciprocal", w=[rden], r=[den], out=rden[:, 0:1], in_=den[:, 0:1])
            comb = combp.get()
            P.e("dve", "tensor_scalar", w=[comb], r=[wsc, rden], out=comb[:], in0=wsc[:], scalar1=rden[:, 0:1],
                scalar2=None, op0=ALU.mult)
            P.dma("act", comb_d[r0:r0 + 128, :], comb[:], r=[comb])
        P.dma("act", hfT_d[:, :, st * 512:(st + 1) * 512].rearrange("c p t -> p c t"), hfT[:], r=[hfT])
    P.end()


def phase_fb(P, D, L, NTOK=4096, NEXP=16):
    P.begin()
    BLK = min(2048, NTOK)
    NBLK = NTOK // BLK
    NT = BLK // 128
    g2bc = load_const(P, "g2bc", D["modbc%d" % L][:, 5120:6144], [128, 1024], F32)
    stage = RPool(P, "wst", 3, [128, 1024], F32, dma=True)
    wgp = RPool(P, "wg", 2, [128, 8, 512], BF16)
    wup = RPool(P, "wu", 2, [128, 8, 512], BF16)
    wdp = RPool(P, "wd", 2, [128, 4, 1024], BF16)
    hfT = P.sb("hfT", [128, 8, BLK], BF16, dma=True)
    comb = P.sb("comb", [128, NT, 16], F32, dma=True)
    ACC = P.sb("ACC", [128, NT, 1024], F32)
    sgp = RPool(P, "sg", 2, [128, 512], F32)
    hidp = RPool(P, "hid", 2, [128, 4, 512], BF16)
    hmp = RPool(P, "hm", 2, [128, 1024], F32, dma=True)
    outp = RPool(P, "out", 2, [128, 1024], F32, dma=True)
    hmid_d, hfT_d, comb_d, hout_d = D["hmid%d" % L], D["hfT%d" % L], D["comb%d" % L], D["hout%d" % L]
    wg_d, wu_d, wd_d = D["moe_wg%d" % L], D["moe_wu%d" % L], D["moe_wd%d" % L]
    for blk in range(NBLK):
        b0 = blk * BLK
        for kc in range(8):
            P.dma("sp", hfT[:, kc, :], hfT_d[kc, :, b0:b0 + BLK], w=[hfT])
        P.dma("sp", comb[:], comb_d[b0:b0 + BLK, :].rearrange("(t p) e -> p t e", p=128), w=[comb])
        for e in range(NEXP):
            wg, wu, wd = wgp.get(), wup.get(), wdp.get()
            load_w_bf16(P, wg, wg_d[e], 1024, 512, stage)
            load_w_bf16(P, wu, wu_d[e], 1024, 512, stage)
            load_w_bf16(P, wd, wd_d[e], 512, 1024, stage)
            for st in range(BLK // 512):
                sc_ = slice(st * 512, (st + 1) * 512)
                hid = hidp.get()
                for f in range(4):
                    fs = slice(f * 128, (f + 1) * 128)
                    gps, ups = P.psum(), P.psum()
                    for kc in range(8):
                        P.mm(gps, gps[:, :], [wg, hfT], wg[:, kc, fs], hfT[:, kc, sc_], start=(kc == 0), stop=(kc == 7))
                    for kc in range(8):
                        P.mm(ups, ups[:, :], [wu, hfT], wu[:, kc, fs], hfT[:, kc, sc_], start=(kc == 0), stop=(kc == 7))
                    sg = sgp.get()
                    P.e("act", "activation", w=[sg], r=[gps], out=sg[:], in_=gps[:, :], func=AF.Silu)
                    P.e("dve", "tensor_tensor", w=[hid], r=[sg, ups], out=hid[:, f, :], in0=ups[:, :], in1=sg[:],
                        op=ALU.mult)
                for t in range(4):
                    ti = st * 4 + t
                    tc = slice(t * 128, (t + 1) * 128)
                    for hf in range(2):
                        ops_ = P.psum()
                        for f in range(4):
                            P.mm(ops_, ops_[:, :], [hid, wd], hid[:, f, tc], wd[:, f, hf * 512:(hf + 1) * 512],
                                 start=(f == 0), stop=(f == 3))
                        hs = slice(hf * 512, (hf + 1) * 512)
                        ccol = comb[:, ti, e:e + 1]
                        if e == 0:
                            P.e("dve", "tensor_scalar", w=[ACC], r=[ops_, comb], out=ACC[:, ti, hs], in0=ops_[:, :],
                                scalar1=ccol, scalar2=None, op0=ALU.mult)
                        else:
                            P.e("dve", "scalar_tensor_tensor", w=[ACC], r=[ops_, comb, ACC], out=ACC[:, ti, hs],
                                in0=ops_[:, :], scalar=ccol, in1=ACC[:, ti, hs], op0=ALU.mult, op1=ALU.add)
        for ti in range(NT):
            r0 = b0 + ti * 128
            hm = hmp.get()
            P.dma("sp", hm[:], hmid_d[r0:r0 + 128, :], w=[hm])
            o = outp.get()
            P.e("pool", "tensor_tensor", w=[o], r=[ACC, g2bc], out=o[:], in0=ACC[:, ti, :], in1=g2bc[:], op=ALU.mult)
            P.e("pool", "tensor_tensor", w=[o], r=[o, hm], out=o[:], in0=o[:], in1=hm[:], op=ALU.add)
            P.dma("act", hout_d[r0:r0 + 128, :], o[:], r=[o])
    P.end()


def phase_m1a(P, D, S_TOK=8192):
    P.begin()
    NST = S_TOK // 512
    identf = load_const(P, "identf", D["ident_f"], [128, 128], F32)
    identb = load_const(P, "identb", D["ident_b"], [128, 128], BF16)
    modcol = load_const(P, "modcol", D["modcol1"], [128, 32], F32)
    convw = load_const(P, "convw", D["lconvw"], [128, 16], F32)
    convb = load_const(P, "convb", D["lconvb"], [128, 4], F32)
    ba = load_const(P, "ba", D["lba"], [128, 4], F32)
    bx = load_const(P, "bx", D["lbx"], [128, 4], F32)
    cneg = load_const(P, "cneg", D["llam"], [128, 4], F32)
    P.e("act", "activation", w=[cneg], r=[cneg], out=cneg[:], in_=cneg[:], func=AF.Exp, scale=-1.0)
    P.e("act", "activation", w=[cneg], r=[cneg], out=cneg[:], in_=cneg[:], func=AF.Ln, bias=1.0)
    P.e("dve", "tensor_scalar", w=[cneg], r=[cneg], out=cneg[:], in0=cneg[:], scalar1=-8.0, scalar2=None, op0=ALU.mult)
    qnc = load_const(P, "qnc", D["qn_col"], [128, 3], F32)
    kvnc = load_const(P, "kvnc", D["kvn_col"], [128, 2], F32)
    gq = load_const(P, "gq", D["gq_row"].to_broadcast([128, 192]), [128, 192], F32)
    P.e("dve", "tensor_scalar", w=[gq], r=[gq], out=gq[:], in0=gq[:], scalar1=192.0 ** -0.5, scalar2=None, op0=ALU.mult)
    gk = load_const(P, "gk", D["gk_row"].to_broadcast([128, 192]), [128, 192], F32)
    freq = load_const(P, "freq", D["freq_row"].to_broadcast([128, 32]), [128, 32], F32)
    stage = RPool(P, "wst", 2, [128, 1024], F32, dma=True)
    wfm = P.sb("wfm", [128, 8, 1024], BF16)
    wtm = P.sb("wtm", [128, 8, 704], BF16)
    wq = P.sb("wq", [128, 3, 768], BF16)
    wkv = P.sb("wkv", [128, 2, 1024], BF16)
    wabd = P.sb("wabd", [128, 4, 128], BF16)
    wxbd = P.sb("wxbd", [128, 4, 128], BF16)
    load_w_bf16(P, wfm, D["w_fm1"], 1024, 1024, stage)
    load_w_bf16(P, wtm, D["w_tm1"], 1024, 704, stage)
    load_w_bf16(P, wq, D["w_q"], 384, 768, stage)
    load_w_bf16(P, wkv, D["w_kv"], 256, 1024, stage)
    load_w_bf16(P, wabd, D["w_abd"], 512, 128, stage)
    load_w_bf16(P, wxbd, D["w_xbd"], 512, 128, stage)
    hst = P.sb("hst", [128, 4], F32)
    P.e("pool", "memset", w=[hst], ap=hst[:], constant=0.0)
    xrb = [P.sb("xrb%d" % i, [128, 515], F32) for i in range(4)]
    for b in xrb:
        P.e("pool", "memset", w=[b], ap=b[:], constant=0.0)
    xpool = RPool(P, "xt", 2, [128, 1024], F32, dma=True)
    junk = RPool(P, "junk", 2, [128, 1024], BF16)
    small = RPool(P, "small", 16, [128, 1], F32)
    xnp = RPool(P, "xn", 1, [128, 1024], F32)
    hmp = RPool(P, "hmT", 2, [128, 8, 512], BF16)
    f512 = RPool(P, "f512", 8, [128, 512], F32)
    b512 = RPool(P, "b512", 3, [128, 512], BF16)
    olp = RPool(P, "ol", 2, [128, 4, 512], BF16, dma=True)
    posp = RPool(P, "pos", 2, [128, 1], I32, dma=True)
    r32 = RPool(P, "r32", 10, [128, 32], F32)
    i32p = RPool(P, "i32", 2, [128, 32], I32)
    un_p = RPool(P, "un", 2, [128, 640], BF16)
    uT_p = RPool(P, "uT", 2, [128, 5, 128], BF16)
    q4p = RPool(P, "q4", 3, [128, 4, 192], F32)
    ropp = RPool(P, "rop", 4, [128, 4, 32], F32)
    s4p = RPool(P, "s4", 8, [128, 4], F32)
    qbp = RPool(P, "qb", 3, [128, 4, 192], BF16)
    qTp = RPool(P, "qT", 2, [128, 4, 2, 128], BF16, dma=True)
    kTp = RPool(P, "kT", 2, [128, 4, 2, 128], BF16, dma=True)
    vbp = RPool(P, "vb", 2, [128, 4, 128], BF16, dma=True)
    xv, pos_d = D["hin1f"], D["pos_col"]
    mixT_d, qT_d, kT_d, v_d = D["mixT1"], D["qT"], D["kT"], D["vtok"]
    TWO_PI = 2.0 * math.pi
    for st in range(NST):
        hmT = hmp.get()
        for t in range(4):
            xt = xpool.get()
            r0 = (st * 4 + t) * 128
            P.dma("sp", xt[:], xv[r0:r0 + 128, :], w=[xt])
            modulate_tile(P, xt, modcol, 0, identf, hmT, t * 128, (junk, small, xnp))
        ol = olp.get()
        for ci in range(4):
            pj = P.psum()
            for kc in range(8):
                P.mm(pj, pj[:, :], [wfm, hmT], wfm[:, kc, 512 + ci * 128:512 + (ci + 1) * 128], hmT[:, kc, :],
                     start=(kc == 0), stop=(kc == 7))
            xb = xrb[ci]
            P.e("pool", "tensor_copy", w=[xb], r=[xb], out=xb[:, 0:3], in_=xb[:, 512:515])
            P.e("act", "activation", w=[xb], r=[pj], out=xb[:, 3:515], in_=pj[:, :], func=AF.Copy)
            acc = f512.get()
            P.e("pool", "tensor_scalar", w=[acc], r=[xb, convw, convb], out=acc[:], in0=xb[:, 0:512],
                scalar1=convw[:, ci * 4:ci * 4 + 1], scalar2=convb[:, ci:ci + 1], op0=ALU.mult, op1=ALU.add)
            for k in range(1, 4):
                tmpc = f512.get()
                P.e("pool", "tensor_scalar", w=[tmpc], r=[xb, convw], out=tmpc[:], in0=xb[:, k:k + 512],
                    scalar1=convw[:, ci * 4 + k:ci * 4 + k + 1], scalar2=None, op0=ALU.mult)
                P.e("pool", "tensor_tensor", w=[acc], r=[tmpc, acc], out=acc[:], in0=acc[:], in1=tmpc[:], op=ALU.add)
            xcb = b512.get()
            P.e("act", "activation", w=[xcb], r=[acc], out=xcb[:], in_=acc[:], func=AF.Copy)
            pa = P.psum()
            P.mm(pa, pa[:, :], [wabd, xcb], wabd[:, ci, :], xcb[:])
            px = P.psum()
            P.mm(px, px[:, :], [wxbd, xcb], wxbd[:, ci, :], xcb[:])
            rr = f512.get()
            P.e("act", "activation", w=[rr], r=[pa, ba], out=rr[:], in_=pa[:, :], func=AF.Sigmoid, bias=ba[:, ci:ci + 1])
            ii = f512.get()
            P.e("act", "activation", w=[ii], r=[px, bx], out=ii[:], in_=px[:, :], func=AF.Sigmoid, bias=bx[:, ci:ci + 1])
            aa = f512.get()
            P.e("act", "activation", w=[aa], r=[rr, cneg], out=aa[:], in_=rr[:], func=AF.Exp, scale=cneg[:, ci:ci + 1])
            a2 = rr
            P.e("pool", "tensor_tensor", w=[a2], r=[aa], out=a2[:], in0=aa[:], in1=aa[:], op=ALU.mult)
            P.e("act", "activation", w=[a2], r=[a2], out=a2[:], in_=a2[:], func=AF.Sqrt, scale=-1.0, bias=1.0)
            P.e("pool", "tensor_tensor", w=[ii], r=[ii, acc], out=ii[:], in0=ii[:], in1=acc[:], op=ALU.mult)
            P.e("pool", "tensor_tensor", w=[ii], r=[ii, a2], out=ii[:], in0=ii[:], in1=a2[:], op=ALU.mult)
            hs_ = f512.get()
            P.e("dve", "tensor_tensor_scan", w=[hs_], r=[aa, ii, hst], out=hs_[:], data0=aa[:], data1=ii[:],
                initial=hst[:, ci:ci + 1], op0=ALU.mult, op1=ALU.add)
            P.e("dve", "tensor_copy", w=[hst], r=[hs_], out=hst[:, ci:ci + 1], in_=hs_[:, 511:512])
            pgt = P.psum()
            for kc in range(8):
                P.mm(pgt, pgt[:, :], [wfm, hmT], wfm[:, kc, ci * 128:(ci + 1) * 128], hmT[:, kc, :],
                     start=(kc == 0), stop=(kc == 7))
            gl = f512.get()
            P.e("act", "activation", w=[gl], r=[pgt], out=gl[:], in_=pgt[:, :], func=AF.Gelu_apprx_tanh)
            P.e("dve", "tensor_tensor", w=[ol], r=[gl, hs_], out=ol[:, ci, :], in0=gl[:], in1=hs_[:], op=ALU.mult)
        P.dma("act", mixT_d[0:4, :, st * 512:(st + 1) * 512].rearrange("c p t -> p c t"), ol[:], r=[ol])
        for t in range(4):
            tc = slice(t * 128, (t + 1) * 128)
            r0 = (st * 4 + t) * 128
            posi = posp.get()
            P.dma("sp", posi[:], pos_d[r0:r0 + 128, :], w=[posi])
            posf = small.get()
            P.e("dve", "tensor_copy", w=[posf], r=[posi], out=posf[:, 0:1], in_=posi[:, 0:1])
            sincos = []
            for off in (0.0, 0.25):
                rr_ = r32.get()
                P.e("dve", "tensor_scalar", w=[rr_], r=[freq, posf], out=rr_[:], in0=freq[:], scalar1=posf[:, 0:1],
                    scalar2=off, op0=ALU.mult, op1=ALU.add)
                ri = i32p.get()
                P.e("dve", "tensor_copy", w=[ri], r=[rr_], out=ri[:], in_=rr_[:])
                rf = r32.get()
                P.e("dve", "tensor_copy", w=[rf], r=[ri], out=rf[:], in_=ri[:])
                fr = r32.get()
                P.e("dve", "tensor_tensor", w=[fr], r=[rr_, rf], out=fr[:], in0=rr_[:], in1=rf[:], op=ALU.subtract)
                sc_ = r32.get()
                P.e("act", "activation", w=[sc_], r=[fr], out=sc_[:], in_=fr[:], func=AF.Sin, scale=TWO_PI)
                sincos.append(sc_)
            sin_, cos_ = sincos
            pA = P.psum()
            for kc in range(8):
                P.mm(pA, pA[:, 0:384], [wtm, hmT], hmT[:, kc, tc], wtm[:, kc, 0:384], start=(kc == 0), stop=(kc == 7))
            pB = P.psum()
            for kc in range(8):
                P.mm(pB, pB[:, 0:320], [wtm, hmT], hmT[:, kc, tc], wtm[:, kc, 384:704], start=(kc == 0), stop=(kc == 7))
            un = un_p.get()
            for (pp, c0, n, o0) in ((pA, 0, 384, 0), (pB, 0, 256, 384)):
                jk = junk.get()
                ss = small.get()
                P.e("act", "activation", w=[jk, ss], r=[pp], out=jk[:, 0:n], in_=pp[:, c0:c0 + n], func=AF.Square,
                    accum_out=ss[:, 0:1])
                rs = rstd_of(P, ss, n, small)
                P.e("dve", "tensor_scalar", w=[un], r=[pp, rs], out=un[:, o0:o0 + n], in0=pp[:, c0:c0 + n],
                    scalar1=rs[:, 0:1], scalar2=None, op0=ALU.mult)
            tpu = P.psum()
            tpuv = tpu[:, :].bitcast(BF16)
            for j in range(5):
                P.tr(tpu, tpuv[:, j * 128:(j + 1) * 128], [un, identb], un[:, j * 128:(j + 1) * 128], identb[:],
                     inc=(j == 4))
            uT = uT_p.get()
            for j in range(5):
                col = qnc[:, j:j + 1] if j < 3 else kvnc[:, j - 3:j - 2]
                P.e("dve", "tensor_scalar", w=[uT], r=[tpu, qnc, kvnc], out=uT[:, j, :], in0=tpuv[:, j * 128:(j + 1) * 128],
                    scalar1=col, scalar2=None, op0=ALU.mult)
            q4 = q4p.get()
            for hf in range(2):
                pq = P.psum()
                for kc in range(3):
                    P.mm(pq, pq[:, 0:384], [uT, wq], uT[:, kc, :], wq[:, kc, hf * 384:(hf + 1) * 384],
                         start=(kc == 0), stop=(kc == 2))
                P.e("act", "activation", w=[q4], r=[pq], out=q4[:, hf * 2:(hf + 1) * 2, :],
                    in_=pq[:, 0:384].rearrange("p (h d) -> p h d", h=2), func=AF.Copy)
            k4 = q4p.get()
            vb = vbp.get()
            for hf in range(2):
                pk = P.psum()
                for kc in range(2):
                    P.mm(pk, pk[:, :], [uT, wkv], uT[:, 3 + kc, :], wkv[:, kc, hf * 512:(hf + 1) * 512],
                         start=(kc == 0), stop=(kc == 1))
                pk3 = pk[:, :].rearrange("p (h d) -> p h d", h=2)
                P.e("act", "activation", w=[k4], r=[pk], out=k4[:, hf * 2:(hf + 1) * 2, 0:128], in_=pk3[:, :, 0:128],
                    func=AF.Copy)
                P.e("dve", "tensor_copy", w=[vb], r=[pk], out=vb[:, hf * 2:(hf + 1) * 2, :], in_=pk3[:, :, 128:256])
            P.dma("act", v_d[:, r0:r0 + 128, :].rearrange("h t e -> t h e"), vb[:], r=[vb])
            P.e("dve", "tensor_copy", w=[k4], r=[pB], out=k4[:, :, 128:192],
                in_=pB[:, 256:320].unsqueeze(1).to_broadcast([128, 4, 64]))
            outs = []
            for (x4, gw_, dst_pool) in ((q4, gq, qTp), (k4, gk, kTp)):
                sq = q4p.get()
                P.e("pool", "tensor_tensor", w=[sq], r=[x4], out=sq[:], in0=x4[:], in1=x4[:], op=ALU.mult)
                ss4 = s4p.get()
                P.e("dve", "tensor_reduce", w=[ss4], r=[sq], out=ss4[:], in_=sq[:], axis=AX.X, op=ALU.add)
                sd4 = s4p.get()
                P.e("act", "activation", w=[sd4], r=[ss4], out=sd4[:], in_=ss4[:], func=AF.Sqrt, scale=1.0 / 192.0, bias=EPS)
                rs4 = s4p.get()
                P.e("dve", "reciprocal", w=[rs4], r=[sd4], out=rs4[:], in_=sd4[:])
                P.e("dve", "tensor_tensor", w=[x4], r=[x4, rs4], out=x4[:], in0=x4[:],
                    in1=rs4[:].unsqueeze(2).to_broadcast([128, 4, 192]), op=ALU.mult)
                P.e("pool", "tensor_tensor", w=[x4], r=[x4, gw_], out=x4[:], in0=x4[:],
                    in1=gw_[:].unsqueeze(1).to_broadcast([128, 4, 192]), op=ALU.mult)
                xb4 = qbp.get()
                P.e("act", "activation", w=[xb4], r=[x4], out=xb4[:, :, 0:128], in_=x4[:, :, 0:128], func=AF.Copy)
                cb_ = cos_[:].unsqueeze(1).to_broadcast([128, 4, 32])
                sb_ = sin_[:].unsqueeze(1).to_broadcast([128, 4, 32])
                x1, x2 = x4[:, :, 128:160], x4[:, :, 160:192]
                ta, tb = ropp.get(), ropp.get()
                P.e("dve", "tensor_tensor", w=[ta], r=[x4, cos_], out=ta[:], in0=x1, in1=cb_, op=ALU.mult)
                P.e("dve", "tensor_tensor", w=[tb], r=[x4, sin_], out=tb[:], in0=x2, in1=sb_, op=ALU.mult)
                P.e("dve", "tensor_tensor", w=[xb4], r=[ta, tb], out=xb4[:, :, 128:160], in0=ta[:], in1=tb[:], op=ALU.subtract)
                tc_, td = ropp.get(), ropp.get()
                P.e("dve", "tensor_tensor", w=[tc_], r=[x4, sin_], out=tc_[:], in0=x1, in1=sb_, op=ALU.mult)
                P.e("dve", "tensor_tensor", w=[td], r=[x4, cos_], out=td[:], in0=x2, in1=cb_, op=ALU.mult)
                P.e("dve", "tensor_tensor", w=[xb4], r=[tc_, td], out=xb4[:, :, 160:192], in0=tc_[:], in1=td[:], op=ALU.add)
                xT = dst_pool.get()
                for hp in range(2):
                    tp_ = P.psum()
                    tpv = tp_[:, :].bitcast(BF16)
                    for hh in range(2):
                        h = hp * 2 + hh
                        P.tr(tp_, tpv[:, hh * 256:hh * 256 + 128], [xb4, identb], xb4[:, h, 0:128], identb[:], inc=False)
                        P.tr(tp_, tpv[0:64, hh * 256 + 128:hh * 256 + 256], [xb4, identb], xb4[:, h, 128:192], identb[:],
                             inc=(hh == 1))
                    tp4 = tpv[:, 0:512].rearrange("p (h c t) -> p h c t", h=2, c=2)
                    P.e("act", "activation", w=[xT], r=[tp_], out=xT[:, hp * 2:hp * 2 + 2, 0, :], in_=tp4[:, :, 0, :],
                        func=AF.Copy)
                    P.e("dve", "tensor_copy", w=[xT], r=[tp_], out=xT[0:64, hp * 2:hp * 2 + 2, 1, :],
                        in_=tpv[0:64, 0:512].rearrange("p (h c t) -> p h c t", h=2, c=2)[:, :, 1, :])
                outs.append(xT)
            qT, kT = outs
            for (xT, dd) in ((qT, qT_d), (kT, kT_d)):
                P.dma("act", dd[:, 0:128, r0:r0 + 128].rearrange("h d t -> d h t"), xT[:, :, 0, :], r=[xT])
                P.dma("act", dd[:, 128:192, r0:r0 + 128].rearrange("h d t -> d h t"), xT[0:64, :, 1, :], r=[xT])
    P.end()


def phase_m1b(P, D, S_TOK=8192, NH=4):
    P.begin()
    NT = S_TOK // 128
    NSB = S_TOK // 512
    identb = load_const(P, "identb", D["ident_b"], [128, 128], BF16)
    maskb = load_const(P, "maskb", D["maskneg_b"], [128, 128], BF16)
    kTa_p = RPool(P, "kTa", 2, [128, S_TOK], BF16, dma=True)
    kTb_p = RPool(P, "kTb", 2, [64, S_TOK], BF16, dma=True)
    V_p = RPool(P, "V", 2, [128, NT, 130], BF16, dma=True)
    qa_p = RPool(P, "qa", 2, [128, 512], BF16, dma=True)
    qb_p = RPool(P, "qb", 2, [64, 512], BF16, dma=True)
    pt_p = RPool(P, "pt", 3, [128, 512], BF16)
    small = RPool(P, "small", 8, [128, 1], F32)
    ob_p = RPool(P, "ob", 2, [128, 128], BF16)
    oT_p = RPool(P, "oT", 2, [128, 512], BF16, dma=True)
    sbank = [P.ps("sbk%d" % i, [128, 512], F32) for i in range(3)]
    obank = [P.ps("obk%d" % i, [128, 512], F32) for i in range(4)]
    tbank = P.ps("tb", [128, 512], F32)
    si = 0
    SHIFT = -8.0
    shiftc = P.sb("shiftc", [128, 1], F32)
    P.e("pool", "memset", w=[shiftc], ap=shiftc[:], constant=SHIFT)
    qT_d, kT_d, v_d, mixT_d = D["qT"], D["kT"], D["vtok"], D["mixT1"]
    for h in range(NH):
        kTa, kTb, V = kTa_p.get(), kTb_p.get(), V_p.get()
        for c in range(4):
            cs = slice(c * (S_TOK // 4), (c + 1) * (S_TOK // 4))
            P.dma("sp", kTa[:, cs], kT_d[h, 0:128, cs], w=[kTa])
        P.dma("sp", kTb[:], kT_d[h, 128:192, :], w=[kTb])
        P.e("pool", "memset", w=[V], ap=V[:, :, 128:130], constant=1.0)
        P.dma("sp", V[:, :, 0:128], v_d[h].rearrange("(t p) e -> p t e", p=128), w=[V])
        for j in range(NSB):
            qa, qb = qa_p.get(), qb_p.get()
            P.dma("sp", qa[:], qT_d[h, 0:128, j * 512:(j + 1) * 512], w=[qa])
            P.dma("sp", qb[:], qT_d[h, 128:192, j * 512:(j + 1) * 512], w=[qb])
            nk = 4 * j + 4
            for kt in range(nk):
                a = max(0, kt - 4 * j)
                diag = kt >= 4 * j
                ks = slice(kt * 128, (kt + 1) * 128)
                sT = sbank[si % 3]
                si += 1
                ncol = 512 - a * 128
                P.mm(sT, sT[:, 0:ncol], [kTa, qa], kTa[:, ks], qa[:, a * 128:512], start=True, stop=False)
                P.mm(sT, sT[:, 0:ncol], [kTb, qb], kTb[:, ks], qb[:, a * 128:512], start=False, stop=(not diag))
                if diag:
                    P.mm(sT, sT[:, 0:128], [identb, maskb], identb[:], maskb[:], start=False, stop=True)
                pt = pt_p.get()
                P.e("act", "activation", w=[pt], r=[sT, shiftc], out=pt[:, 0:ncol], in_=sT[:, 0:ncol], func=AF.Exp,
                    bias=shiftc[:, 0:1])
                for qb_i in range(a, 4):
                    ob = obank[qb_i]
                    P.mm(ob, ob[:, 0:130], [pt, V], pt[:, (qb_i - a) * 128:(qb_i - a + 1) * 128], V[:, kt, :],
                         start=(kt == 0), stop=(kt == 4 * j + qb_i))
            oT = oT_p.get()
            for qb_i in range(4):
                ob = obank[qb_i]
                rd = small.get()
                P.e("dve", "reciprocal", w=[rd], r=[ob], out=rd[:, 0:1], in_=ob[:, 128:129])
                obf = ob_p.get()
                P.e("dve", "tensor_scalar", w=[obf], r=[ob, rd], out=obf[:], in0=ob[:, 0:128], scalar1=rd[:, 0:1],
                    scalar2=None, op0=ALU.mult)
                tbv = tbank[:, :].bitcast(BF16)
                P.tr(tbank, tbv[:, qb_i * 128:(qb_i + 1) * 128], [obf, identb], obf[:], identb[:])
            P.e("act", "activation", w=[oT], r=[tbank], out=oT[:], in_=tbank[:, :].bitcast(BF16)[:, 0:512], func=AF.Copy)
            P.dma("act", mixT_d[4 + h, :, j * 512:(j + 1) * 512], oT[:], r=[oT])
    P.end()


def m1_inputs(d, b, p, hfull, S_TOK=8192):
    w = d['od_w_in'][0]
    offs = np.cumsum([0, 1024, 1024, 384, 256, 64])
    gate, xr, uq, ukv, kr = [w[:, offs[i]:offs[i + 1]] for i in range(5)]
    cs = slice(p * 512, (p + 1) * 512)
    cw = d['lru_conv_w'][0][:, cs]
    lconvw = np.ascontiguousarray(cw.reshape(4, 4, 128).transpose(2, 1, 0).reshape(128, 16))

    def c4(v):
        return np.ascontiguousarray(np.asarray(v, np.float32)[cs].reshape(4, 128).T)

    def bd(wb):
        out = np.zeros((4, 128, 128), np.float32)
        for ci in range(4):
            for j in range(2):
                out[ci, j * 64:(j + 1) * 64, j * 64:(j + 1) * 64] = wb[p * 8 + ci * 2 + j]
        return out.reshape(512, 128)

    hs = slice(p * 4, p * 4 + 4)
    wq = d['mla_w_q_up'][0].reshape(384, 8, 192)[:, hs].reshape(384, 768)
    wkv = d['mla_w_kv_up'][0].reshape(256, 8, 256)[:, hs].reshape(256, 1024)
    freqs = (10000.0 ** (-np.arange(0, 64, 2, dtype=np.float32) / 64.0)).astype(np.float32)
    j = np.arange(128)
    m = {
        "hin1f": np.ascontiguousarray(hfull[:S_TOK]), "c_col": col128(d['c'][b]),
        "ada_w1": d['ada_w'][1], "ada_b1": d['ada_b'][1][None, :],
        "nm_col1": col128(d['norm_mix'][1]), "nf_col1": col128(d['norm_ffn'][1]),
        "pos_col": np.ascontiguousarray(d['positions'][b, :S_TOK].astype(np.int32).reshape(-1, 1)),
        "w_fm1": np.ascontiguousarray(np.concatenate([gate[:, cs], xr[:, cs]], axis=1)),
        "w_tm1": np.ascontiguousarray(np.concatenate([uq, ukv, kr], axis=1)),
        "w_q": np.ascontiguousarray(wq), "w_kv": np.ascontiguousarray(wkv),
        "w_abd": bd(d['lru_w_a'][0]), "w_xbd": bd(d['lru_w_x'][0]),
        "lconvw": lconvw, "lconvb": c4(d['lru_conv_b'][0]), "lba": c4(d['lru_b_a'][0]), "lbx": c4(d['lru_b_x'][0]),
        "llam": c4(d['lru_lambda'][0]),
        "qn_col": np.ascontiguousarray(d['mla_q_norm'][0].reshape(3, 128).T),
        "kvn_col": np.ascontiguousarray(d['mla_kv_norm'][0].reshape(2, 128).T),
        "gq_row": d['mla_q_qknorm'][0][None, :], "gk_row": d['mla_k_qknorm'][0][None, :],
        "freq_row": (freqs / np.float32(2.0 * math.pi)).astype(np.float32)[None, :],
        "maskneg_b": bf16(np.where(j[:, None] > j[None, :], NEG, 0.0)),
    }
    m.update(host_consts())
    return m


def m0_inputs(d, b, p):
    w = d['ev_w_in'][0]
    offs = np.cumsum([0, 256, 256, 512, 16, 512, 1024, 1536, 16])
    q, k, v, glr, og, z, xbc, dtw = [w[:, offs[i]:offs[i + 1]] for i in range(8)]
    hs = slice(p * 2, p * 2 + 2)
    qh = q.reshape(1024, 4, 64)[:, hs].reshape(1024, 128)
    kh = k.reshape(1024, 4, 64)[:, hs].reshape(1024, 128)
    vh = v.reshape(1024, 4, 128)[:, hs].reshape(1024, 256)
    ogh = og.reshape(1024, 4, 128)[:, hs].reshape(1024, 256)
    zh = z[:, p * 512:(p + 1) * 512]
    xh = xbc[:, p * 512:(p + 1) * 512]
    Bh = xbc[:, 1024 + p * 128: 1024 + (p + 1) * 128]
    Ch = xbc[:, 1280 + p * 128: 1280 + (p + 1) * 128]
    dth = dtw[:, p * 8:(p + 1) * 8]
    cw = d['ssd_conv_w'][0]
    cbias = d['ssd_conv_b'][0]
    chans = np.concatenate([np.arange(p * 512, (p + 1) * 512), 1024 + np.arange(p * 128, (p + 1) * 128),
                            1280 + np.arange(p * 128, (p + 1) * 128)])
    cwh = cw[:, chans]
    convw = np.ascontiguousarray(cwh.reshape(4, 6, 128).transpose(2, 1, 0).reshape(128, 24))
    convb = np.ascontiguousarray(cbias[chans].reshape(6, 128).T)
    m = {
        "x": d['x'][b], "c_col": col128(d['c'][b]),
        "ada_w0": d['ada_w'][0], "ada_b0": d['ada_b'][0][None, :],
        "nm_col0": col128(d['norm_mix'][0]), "nf_col0": col128(d['norm_ffn'][0]),
        "w_fm": np.ascontiguousarray(np.concatenate([qh, kh, xh, Bh, Ch], axis=1)),
        "w_tm": np.ascontiguousarray(np.concatenate([vh, ogh, zh], axis=1)),
        "w_glr": np.ascontiguousarray(glr), "w_dt": np.ascontiguousarray(dth),
        "w_g2": np.ascontiguousarray(d['gla_w_g2'][0].reshape(16, 4, 64)[:, hs].reshape(16, 128)),
        "bg2_col": np.ascontiguousarray(d['gla_b_g2'][0].reshape(4, 64)[hs].reshape(128, 1)),
        "onorm_row": np.ascontiguousarray(d['gla_onorm'][0].reshape(4, 128)[hs].reshape(1, 256)),
        "ssdn_row": np.ascontiguousarray(d['ssd_norm'][0][p * 512:(p + 1) * 512][None, :]),
        "convw": convw, "convb": convb,
        "dtb_col": np.ascontiguousarray(d['ssd_dt_bias'][0][p * 8:(p + 1) * 8].reshape(8, 1)),
        "alog_col": np.ascontiguousarray(d['ssd_a_log'][0][p * 8:(p + 1) * 8].reshape(8, 1)),
        "dskip_row": np.ascontiguousarray(d['ssd_d'][0][p * 8:(p + 1) * 8].reshape(1, 8)),
    }
    m.update(host_consts())
    return m


def f_inputs(d, L, b, p, hin, mixT):
    m = {"hin%d" % L: np.ascontiguousarray(hin), "mixT%d" % L: np.ascontiguousarray(mixT),
         "w_out%d" % L: (d['ev_w_out'][0] if L == 0 else d['od_w_out'][0]),
         "c_col": col128(d['c'][b]), "ada_w%d" % L: d['ada_w'][L], "ada_b%d" % L: d['ada_b'][L][None, :],
         "nm_col%d" % L: col128(d['norm_mix'][L]), "nf_col%d" % L: col128(d['norm_ffn'][L]),
         "router_w": d['router_w'], "router_b": d['router_bias'][None, :],
         "moe_wg%d" % L: d['moe_w_gate'][L], "moe_wu%d" % L: d['moe_w_up'][L], "moe_wd%d" % L: d['moe_w_down'][L]}
    m.update(host_consts())
    return m


def _np_dt(v):
    if v.dtype == ml_dtypes.bfloat16:
        return BF16
    if v.dtype == np.int32:
        return I32
    return F32


def run_launch(in_maps, outs, scratch, phases):
    nc = bass.Bass("TRN2", target_bir_lowering=False)
    D = {}
    for k, v in in_maps[0].items():
        D[k] = nc.dram_tensor(k, list(v.shape), _np_dt(v), kind="ExternalInput").ap()
    for k, (shape, dt) in outs.items():
        D[k] = nc.dram_tensor(k, list(shape), dt, kind="ExternalOutput").ap()
    for k, (shape, dt) in scratch.items():
        D[k] = nc.dram_tensor(k, list(shape), dt).ap()
    P = Prog(nc)
    phases(P, D)
    res = run_bass_kernel_spmd(nc, in_maps, core_ids=list(range(len(in_maps))))
    return res.results


def kernel(**inputs):
    d = {k: np.asarray(v) for k, v in inputs.items()}
    S, NT_ = 8192, 4096
    cores = [(c // 2, c % 2) for c in range(8)]
    MOD = {"modbc%d": ([128, 6144], F32), "modcol%d": ([128, 32], F32)}

    def mod(L):
        return {k % L: v for k, v in MOD.items()}

    def fscr(L):
        s = mod(L)
        s.update({"hmid%d" % L: ([NT_, 1024], F32), "hfT%d" % L: ([8, 128, NT_], BF16), "comb%d" % L: ([NT_, 16], F32)})
        return s

    r1 = run_launch([m0_inputs(d, b, p) for b, p in cores], {"mixT0": ([6, 128, S], BF16)}, mod(0),
                    lambda P, D: (phase_ada(P, D, 0), phase_m0(P, D, S)))
    maps = []
    for b, p in cores:
        a0, a1 = r1[2 * b]["mixT0"], r1[2 * b + 1]["mixT0"]
        full = np.concatenate([a0[0:2], a1[0:2], a0[2:6], a1[2:6]], axis=0)
        t0 = p * NT_
        maps.append(f_inputs(d, 0, b, p, d['x'][b, t0:t0 + NT_], full[:, :, t0:t0 + NT_]))
    r2 = run_launch(maps, {"hout0": ([NT_, 1024], F32)}, fscr(0),
                    lambda P, D: (phase_ada(P, D, 0), phase_fa(P, D, 0, 12, NT_), phase_fb(P, D, 0, NT_)))
    h0 = [np.concatenate([r2[2 * b]["hout0"], r2[2 * b + 1]["hout0"]], axis=0) for b in range(4)]
    scr = mod(1)
    scr.update({"qT": ([4, 192, S], BF16), "kT": ([4, 192, S], BF16), "vtok": ([4, S, 128], BF16)})
    r3 = run_launch([m1_inputs(d, b, p, h0[b], S) for b, p in cores], {"mixT1": ([8, 128, S], BF16)}, scr,
                    lambda P, D: (phase_ada(P, D, 1), phase_m1a(P, D, S), phase_m1b(P, D, S)))
    maps = []
    for b, p in cores:
        a0, a1 = r3[2 * b]["mixT1"], r3[2 * b + 1]["mixT1"]
        full = np.concatenate([a0[0:4], a1[0:4], a0[4:8], a1[4:8]], axis=0)
        t0 = p * NT_
        maps.append(f_inputs(d, 1, b, p, h0[b][t0:t0 + NT_], full[:, :, t0:t0 + NT_]))
    r4 = run_launch(maps, {"hout1": ([NT_, 1024], F32)}, fscr(1),
                    lambda P, D: (phase_ada(P, D, 1), phase_fa(P, D, 1, 16, NT_), phase_fb(P, D, 1, NT_)))
    out = np.stack([np.concatenate([r4[2 * b]["hout1"], r4[2 * b + 1]["hout1"]], axis=0) for b in range(4)], axis=0)
    return out.astype(np.float32)
```
